# Optimizing a Trainium2 kernel written in Bass

```python
import math
import jax, jax.numpy as jnp
from jax import lax
import numpy as np

D_MODEL = 1024
BATCH = 4
SEQ = 4096
DEPTH = 2
DEC_BATCH = 128
DEC_SEQ = 8
PAST_LEN = 8192
PAGE_SIZE = 128

POOL_WIDTH = D_MODEL // 4
POOL_GROUPS = 4
POOL_GROUP_DIM = POOL_WIDTH // POOL_GROUPS
POOL_WINDOWS = (2, 4, 8, 16)
POOL_BUF = max(POOL_WINDOWS) - 1
HEAD_DIM = 64
N_Q_HEADS = (D_MODEL - POOL_WIDTH) // HEAD_DIM
N_KV_HEADS = 4
GQA_GROUP = N_Q_HEADS // N_KV_HEADS
ATTN_WIDTH = N_Q_HEADS * HEAD_DIM
KV_WIDTH = N_KV_HEADS * HEAD_DIM
MIX_WIDTH = POOL_WIDTH + ATTN_WIDTH
IN_WIDTH = POOL_WIDTH + ATTN_WIDTH + 2 * KV_WIDTH
WINDOW = 128
ATTN_BLOCK = 128
D_FF = 2816
N_EXPERTS = 8
TOP_K = 2
D_FF_EXPERT = 2816
N_DENSE = (DEPTH + 1) // 2
N_MOE = DEPTH // 2
EPS = 1e-6

kernel_name = "hymba_pool_swa_alibi_moe_step"


def alibi_slopes(n):
    def pow2_slopes(m):
        start = 2.0 ** (-8.0 / m)
        return [start ** (i + 1) for i in range(m)]
    if float(math.log2(n)).is_integer():
        s = pow2_slopes(n)
    else:
        c = 2 ** int(math.floor(math.log2(n)))
        s = pow2_slopes(c) + pow2_slopes(2 * c)[0::2][: n - c]
    return jnp.asarray(np.array(s, dtype=np.float32))


def rms_norm(x, g):
    xf = x.astype(jnp.float32)
    y = xf * lax.rsqrt(jnp.mean(xf * xf, axis=-1, keepdims=True) + EPS) * g.astype(jnp.float32)
    return y.astype(x.dtype)


def project(h, w_in, q_gain, k_gain):
    proj = h @ w_in
    lead = proj.shape[:-1]
    u = proj[..., :POOL_WIDTH]
    o = POOL_WIDTH
    q = proj[..., o:o + ATTN_WIDTH].reshape(lead + (N_Q_HEADS, HEAD_DIM))
    o += ATTN_WIDTH
    k = proj[..., o:o + KV_WIDTH].reshape(lead + (N_KV_HEADS, HEAD_DIM))
    o += KV_WIDTH
    v = proj[..., o:o + KV_WIDTH].reshape(lead + (N_KV_HEADS, HEAD_DIM))
    return u, rms_norm(q, q_gain), rms_norm(k, k_gain), v


def pool_mix(u_ext, start_pos, pool_w, pool_scale):
    N = u_ext.shape[0]
    T = u_ext.shape[1] - POOL_BUF
    uf = u_ext.astype(jnp.float32)
    cs = jnp.pad(jnp.cumsum(uf, axis=1), ((0, 0), (1, 0), (0, 0)))
    u_tok = uf[:, POOL_BUF:]
    pos = start_pos + jnp.arange(T)
    outs = []
    for g, w in enumerate(POOL_WINDOWS):
        sl = slice(g * POOL_GROUP_DIM, (g + 1) * POOL_GROUP_DIM)
        win = cs[:, POOL_BUF + 1:POOL_BUF + T + 1, sl] - cs[:, POOL_BUF + 1 - w:POOL_BUF + T + 1 - w, sl]
        cnt = jnp.minimum(w, pos + 1).astype(jnp.float32)[None, :, None]
        outs.append(win / cnt - u_tok[..., sl])
    pooled = jnp.stack(outs, axis=2).astype(u_ext.dtype)
    mixed = jnp.einsum('ntgc,gcd->ntgd', pooled, pool_w).reshape(N, T, POOL_WIDTH)
    return mixed * pool_scale


def attend(q, k, v, dist, valid, slopes, sinks):
    qg = q.reshape(q.shape[:-2] + (N_KV_HEADS, GQA_GROUP, HEAD_DIM))
    s = jnp.einsum('...qhgd,...khd->...hgqk', qg, k, preferred_element_type=jnp.float32) * (HEAD_DIM ** -0.5)
    sl = slopes.reshape(N_KV_HEADS, GQA_GROUP)[:, :, None, None]
    s = s - sl * dist[..., None, None, :, :]
    s = jnp.where(valid[..., None, None, :, :], s, -jnp.inf)
    sink = sinks.astype(jnp.float32).reshape(N_KV_HEADS, GQA_GROUP)[:, :, None]
    m = jnp.maximum(jnp.max(s, axis=-1), sink)
    p = jnp.exp(s - m[..., None])
    denom = jnp.sum(p, axis=-1) + jnp.exp(sink - m)
    pr = (p / denom[..., None]).astype(v.dtype)
    o = jnp.einsum('...hgqk,...khd->...qhgd', pr, v)
    return o.reshape(o.shape[:-3] + (ATTN_WIDTH,))


def attn_prompt(q, k, v, slopes, sinks):
    B, S = q.shape[:2]
    NB = S // ATTN_BLOCK
    qb = q.reshape(B, NB, ATTN_BLOCK, N_Q_HEADS, HEAD_DIM)

    def band(x):
        xp = jnp.pad(x, ((0, 0), (ATTN_BLOCK, 0), (0, 0), (0, 0)))
        prev = xp[:, :S].reshape(B, NB, ATTN_BLOCK, N_KV_HEADS, HEAD_DIM)
        cur = x.reshape(B, NB, ATTN_BLOCK, N_KV_HEADS, HEAD_DIM)
        return jnp.concatenate([prev, cur], axis=2)

    kb, vb = band(k), band(v)
    start = jnp.arange(NB)[:, None] * ATTN_BLOCK
    qpos = start + jnp.arange(ATTN_BLOCK)[None, :]
    kpos = start - ATTN_BLOCK + jnp.arange(2 * ATTN_BLOCK)[None, :]
    dist = qpos[:, :, None] - kpos[:, None, :]
    valid = (dist >= 0) & (dist < WINDOW) & (kpos[:, None, :] >= 0)
    o = attend(qb, kb, vb, dist.astype(jnp.float32), valid, slopes, sinks)
    return o.reshape(B, S, ATTN_WIDTH)


def attn_sample(q, k_ext, v_ext, slopes, sinks, n_buf):
    T = q.shape[1]
    qpos = PAST_LEN + jnp.arange(T)
    kpos = PAST_LEN - n_buf + jnp.arange(n_buf + T)
    dist = qpos[:, None] - kpos[None, :]
    valid = (dist >= 0) & (dist < WINDOW)
    return attend(q, k_ext, v_ext, dist.astype(jnp.float32), valid, slopes, sinks)


def prompt_mixers(h, w_in, q_gain, k_gain, slopes, sinks, pool_w, pool_scale):
    u, q, k, v = project(h, w_in, q_gain, k_gain)
    u_ext = jnp.pad(u, ((0, 0), (POOL_BUF, 0), (0, 0)))
    a_pool = pool_mix(u_ext, 0, pool_w, pool_scale)
    a_attn = attn_prompt(q, k, v, slopes, sinks)
    n_keep = min(WINDOW, h.shape[1])
    mix = jnp.concatenate([a_pool, a_attn], axis=-1)
    return mix, u_ext[:, -POOL_BUF:], k[:, -n_keep:], v[:, -n_keep:]


def sample_mixers(h, pool_buf, k_buf, v_buf, w_in, q_gain, k_gain, slopes, sinks, pool_w, pool_scale):
    u, q, k, v = project(h, w_in, q_gain, k_gain)
    u_ext = jnp.concatenate([pool_buf.astype(u.dtype), u], axis=1)
    a_pool = pool_mix(u_ext, PAST_LEN, pool_w, pool_scale)
    n_buf = k_buf.shape[1]
    k_ext = jnp.concatenate([k_buf.astype(k.dtype), k], axis=1)
    v_ext = jnp.concatenate([v_buf.astype(v.dtype), v], axis=1)
    a_attn = attn_sample(q, k_ext, v_ext, slopes, sinks, n_buf)
    mix = jnp.concatenate([a_pool, a_attn], axis=-1)
    return mix, u_ext[:, -POOL_BUF:], k_ext[:, -n_buf:], v_ext[:, -n_buf:]


def swiglu(h, wg, wu, wd):
    return (jax.nn.silu(h @ wg) * (h @ wu)) @ wd


def moe_swiglu(h, router, wg, wu, wd):
    logits = (h @ router).astype(jnp.float32)
    top_v, top_i = lax.top_k(logits, TOP_K)
    gates = jax.nn.softmax(top_v, axis=-1)
    combine = jnp.sum(jax.nn.one_hot(top_i, N_EXPERTS, dtype=jnp.float32) * gates[..., None], axis=-2)
    combine = combine.astype(h.dtype)
    out = jnp.zeros(h.shape, h.dtype)
    for e in range(N_EXPERTS):
        out = out + combine[..., e:e + 1] * swiglu(h, wg[e], wu[e], wd[e])
    return out


def channel_mixer(h, l, ffn_w_gate, ffn_w_up, ffn_w_down, moe_router, moe_w_gate, moe_w_up, moe_w_down):
    i = l // 2
    if l % 2 == 0:
        return swiglu(h, ffn_w_gate[i], ffn_w_up[i], ffn_w_down[i])
    return moe_swiglu(h, moe_router[i], moe_w_gate[i], moe_w_up[i], moe_w_down[i])


def setup_inputs(seed: int = 0) -> dict:
    key = jax.random.key(seed)
    ks = jax.random.split(key, 20)
    f32 = jnp.float32
    n_buf = min(WINDOW, PAST_LEN)

    def nrm(k, shape, scale):
        return jax.random.normal(k, shape, f32) * scale

    return {
        "x_prompt": nrm(ks[0], (BATCH, SEQ, D_MODEL), 1.0),
        "x_sample": nrm(ks[1], (DEC_BATCH, DEC_SEQ, D_MODEL), 1.0),
        "state_pool": nrm(ks[2], (DEPTH, DEC_BATCH, POOL_BUF, POOL_WIDTH), 1.0),
        "state_win_k": nrm(ks[3], (DEPTH, DEC_BATCH, n_buf, N_KV_HEADS, HEAD_DIM), 1.0),
        "state_win_v": nrm(ks[4], (DEPTH, DEC_BATCH, n_buf, N_KV_HEADS, HEAD_DIM), 1.0),
        "norm_mix": 1.0 + nrm(ks[5], (DEPTH, D_MODEL), 0.1),
        "w_in": nrm(ks[6], (DEPTH, D_MODEL, IN_WIDTH), D_MODEL ** -0.5),
        "q_norm": 1.0 + nrm(ks[7], (DEPTH, HEAD_DIM), 0.1),
        "k_norm": 1.0 + nrm(ks[8], (DEPTH, HEAD_DIM), 0.1),
        "attn_sinks": nrm(ks[9], (DEPTH, N_Q_HEADS), 0.5),
        "pool_w": nrm(ks[10], (DEPTH, POOL_GROUPS, POOL_GROUP_DIM, POOL_GROUP_DIM), POOL_GROUP_DIM ** -0.5),
        "pool_scale": 1.0 + nrm(ks[11], (DEPTH, POOL_WIDTH), 0.1),
        "w_out": nrm(ks[12], (DEPTH, MIX_WIDTH, D_MODEL), MIX_WIDTH ** -0.5),
        "norm_ffn": 1.0 + nrm(ks[13], (DEPTH, D_MODEL), 0.1),
        "ffn_w_gate": nrm(ks[14], (N_DENSE, D_MODEL, D_FF), D_MODEL ** -0.5),
        "ffn_w_up": nrm(ks[15], (N_DENSE, D_MODEL, D_FF), D_MODEL ** -0.5),
        "ffn_w_down": nrm(ks[16], (N_DENSE, D_FF, D_MODEL), D_FF ** -0.5),
        "moe_router": nrm(ks[17], (N_MOE, D_MODEL, N_EXPERTS), D_MODEL ** -0.5),
        "moe_w_gate": nrm(ks[18], (N_MOE, N_EXPERTS, D_MODEL, D_FF_EXPERT), D_MODEL ** -0.5),
        "moe_w_up": nrm(jax.random.fold_in(ks[19], 0), (N_MOE, N_EXPERTS, D_MODEL, D_FF_EXPERT), D_MODEL ** -0.5),
        "moe_w_down": nrm(jax.random.fold_in(ks[19], 1), (N_MOE, N_EXPERTS, D_FF_EXPERT, D_MODEL), D_FF_EXPERT ** -0.5),
    }


def reference(x_prompt, x_sample, state_pool, state_win_k, state_win_v,
              norm_mix, w_in, q_norm, k_norm, attn_sinks, pool_w, pool_scale, w_out, norm_ffn,
              ffn_w_gate, ffn_w_up, ffn_w_down, moe_router, moe_w_gate, moe_w_up, moe_w_down):
    slopes = alibi_slopes(N_Q_HEADS)
    xp, xs = x_prompt, x_sample
    pool_p, k_p, v_p, pool_s, k_s, v_s = [], [], [], [], [], []
    for l in range(DEPTH):
        h = rms_norm(xp, norm_mix[l])
        mix, pu, pk, pv = prompt_mixers(h, w_in[l], q_norm[l], k_norm[l], slopes, attn_sinks[l], pool_w[l], pool_scale[l])
        xp = xp + mix @ w_out[l]
        xp = xp + channel_mixer(rms_norm(xp, norm_ffn[l]), l, ffn_w_gate, ffn_w_up, ffn_w_down,
                                moe_router, moe_w_gate, moe_w_up, moe_w_down)
        pool_p.append(pu); k_p.append(pk); v_p.append(pv)
        h = rms_norm(xs, norm_mix[l])
        mix, su, sk, sv = sample_mixers(h, state_pool[l], state_win_k[l], state_win_v[l], w_in[l], q_norm[l], k_norm[l],
                                        slopes, attn_sinks[l], pool_w[l], pool_scale[l])
        xs = xs + mix @ w_out[l]
        xs = xs + channel_mixer(rms_norm(xs, norm_ffn[l]), l, ffn_w_gate, ffn_w_up, ffn_w_down,
                                moe_router, moe_w_gate, moe_w_up, moe_w_down)
        pool_s.append(su); k_s.append(sk); v_s.append(sv)
    return (xp, xs,
            jnp.stack(pool_p), jnp.stack(k_p), jnp.stack(v_p),
            jnp.stack(pool_s), jnp.stack(k_s), jnp.stack(v_s))
```

```python
import contextlib
import math
import numpy as np
import concourse.bass as bass
import concourse.mybir as mybir
from concourse.bass_utils import run_bass_kernel_spmd

F32 = mybir.dt.float32
BF16 = mybir.dt.bfloat16
AF = mybir.ActivationFunctionType
ALU = mybir.AluOpType

D = 1024
DFF = 2816
NFF = 22
NE = 8
EPS = 1e-6
NCORE = 8
import os
DEBUG = bool(os.environ.get("KDEBUG"))
NTILE = 19
SEGS = [[0, 1, 2, 3, 4, 5, 6, 7, 8, 9], [10, 11, 12, 13, 14, 15, 16, 17, 18]]


def PAR(t):
    return t % 2


def RSLOT(t):
    return t % 3
G = 2
NSLOT = 3
QPAIR = [(0, 3), (1, 4), (2, 5), (6, 9), (7, 10), (8, 11)]
QPOS = {}
for _j, (_a, _b) in enumerate(QPAIR):
    QPOS[_a] = (_j, 0)
    QPOS[_b] = (_j, 64)


def alibi_slopes(n):
    def p2(m):
        start = 2.0 ** (-8.0 / m)
        return [start ** (i + 1) for i in range(m)]
    if float(math.log2(n)).is_integer():
        return p2(n)
    c = 2 ** int(math.floor(math.log2(n)))
    return p2(c) + p2(2 * c)[0::2][: n - c]


SLOPES = [float(np.float32(s)) for s in alibi_slopes(12)]


class Sched:
    ENG = ('pe', 'act', 'dve', 'pool', 'sp')

    def __init__(self, nc, ndma=8):
        self.nc = nc
        self.prog = {e: [] for e in self.ENG}
        self.sems = {}
        self.cnt = {e: 0 for e in self.ENG}
        self.waited = {e: {} for e in self.ENG}
        self.res = {}
        self.bank = {}
        self.ndma = ndma
        self.dma_i = {}
        self.dma_val = {}
        self.stack = []

    def sem(self, key):
        if key not in self.sems:
            cm = self.nc.semaphore("s%d" % len(self.sems))
            self.sems[key] = cm.__enter__()
            self.stack.append(cm)
        return self.sems[key]

    def _need(self, eng, tok, waits, own_ok=True):
        if tok is None:
            return
        k, v = tok
        if k == eng and (eng == 'pe' or not own_ok):
            return
        if self.waited[eng].get(k, 0) >= v:
            return
        self.waited[eng][k] = v
        waits.append((k, v))

    def _deps(self, eng, reads, writes, banks, waits):
        for r in reads:
            st = self.res.setdefault(r, [None, []])
            self._need(eng, st[0], waits)
        for w in writes:
            st = self.res.setdefault(w, [None, []])
            self._need(eng, st[0], waits)
            for t in st[1]:
                self._need(eng, t, waits, own_ok=False)
        for b in banks:
            self._need(eng, self.bank.get(b), waits, own_ok=False)

    def _commit(self, tok, reads, writes, banks):
        for r in reads:
            self.res[r][1].append(tok)
        for w in writes:
            self.res[w] = [tok, []]
        for b in banks:
            self.bank[b] = tok

    def op(self, eng, fn, reads=(), writes=(), banks=(), inc=True):
        waits = []
        self._deps(eng, reads, writes, banks, waits)
        tok = (eng, self.cnt[eng] + 1)
        if inc:
            self.cnt[eng] += 1
        self.prog[eng].append((waits, fn, (eng, 1) if inc else None))
        self._commit(tok, reads, writes, banks)
        return tok

    def dma(self, q, fn, reads=(), writes=(), chan='d'):
        waits = []
        self._deps(q, reads, writes, (), waits)
        i = self.dma_i.get(chan, 0)
        self.dma_i[chan] = i + 1
        key = ('dma', chan, i % self.ndma)
        prev = self.dma_val.get(key, 0)
        if prev:
            self._need(q, (key, prev), waits)
        val = prev + 16
        self.dma_val[key] = val
        tok = (key, val)
        self.prog[q].append((waits, fn, (key, 16)))
        self._commit(tok, reads, writes, ())
        return tok

    def barrier(self):
        toks = [(e, self.cnt[e]) for e in self.ENG if self.cnt[e] > 0]
        toks += [(k, v) for k, v in self.dma_val.items()]
        for e in self.ENG:
            waits = []
            for t in toks:
                self._need(e, t, waits, own_ok=False)
            if waits:
                self.prog[e].append((waits, None, None))

    def emit(self):
        nc = self.nc
        for e in self.ENG:
            self.sem(e)
            for waits, fn, inc in self.prog[e]:
                for k, v in waits:
                    self.sem(k)
                if inc is not None:
                    self.sem(inc[0])
        engobj = {'pe': 'tensor', 'act': 'scalar', 'dve': 'vector', 'pool': 'gpsimd', 'sp': 'sync'}
        with nc.allow_non_contiguous_dma(reason="tiny strided parameter loads"), nc.Block() as block:
            for e in self.ENG:
                def body(engine, e=e):
                    for waits, fn, inc in self.prog[e]:
                        for k, v in waits:
                            engine.wait_ge(self.sems[k], v)
                        if fn is not None:
                            ins = fn(engine)
                            if inc is not None:
                                ins.then_inc(self.sems[inc[0]], inc[1])
                getattr(block, engobj[e])(body)

    def close(self):
        for cm in reversed(self.stack):
            cm.__exit__(None, None, None)


def build():
    nc = bass.Bass("TRN2", target_bir_lowering=False)

    def din(name, shape):
        return nc.dram_tensor(name, list(shape), F32, kind="ExternalInput").ap()

    def dout(name, shape):
        return nc.dram_tensor(name, list(shape), F32, kind="ExternalOutput").ap()

    xin = din("xin", [NTILE * 128, D])
    flag_d = din("flag", [128, 1])
    cdist_d = din("cdist", [128, 392])
    cvalid_d = din("cvalid", [128, 392])
    tab_d = din("tab", [128, 2, 2, 128])
    sp_d = din("st_pool", [2, 240, 256])
    sk_d = din("st_k", [2, 16, 128, 256])
    sv_d = din("st_v", [2, 16, 128, 256])
    norm_mix_d = din("norm_mix", [2, D])
    norm_ffn_d = din("norm_ffn", [2, D])
    w_in_d = din("w_in", [2, D, 1536])
    qn_d = din("q_norm", [2, 64])
    kn_d = din("k_norm", [2, 64])
    sinks_d = din("attn_sinks", [2, 12])
    pool_w_d = din("pool_w", [2, 4, 64, 64])
    pool_scale_d = din("pool_scale", [2, 256])
    w_out_d = din("w_out", [2, D, D])
    fg_d = din("ffn_w_gate", [1, D, DFF])
    fu_d = din("ffn_w_up", [1, D, DFF])
    fd_d = din("ffn_w_down", [1, DFF, D])
    router_d = din("moe_router", [1, D, NE])
    mg_d = din("moe_w_gate", [1, NE, D, DFF])
    mu_d = din("moe_w_up", [1, NE, D, DFF])
    md_d = din("moe_w_down", [1, NE, DFF, D])

    y_d = dout("y", [17 * 128, D])
    pool_p_d = dout("pool_p", [2, 128, 256])
    k_p_d = dout("k_p", [2, 128, 256])
    v_p_d = dout("v_p", [2, 128, 256])
    pool_s_new_d = dout("pool_s_new", [2, 128, 256])
    pool_s_old_d = dout("pool_s_old", [2, 16, 7, 256])
    k_s_new_d = dout("k_s_new", [2, 128, 256])
    k_s_old_d = dout("k_s_old", [2, 16, 120, 256])
    v_s_new_d = dout("v_s_new", [2, 128, 256])
    v_s_old_d = dout("v_s_old", [2, 16, 120, 256])

    dbg_d = dout("dbg", [4, 2 * 128, D]) if DEBUG else None
    dbg_attn = nc.dram_tensor("dbg_attn", [128, 768], BF16, kind="ExternalOutput").ap() if DEBUG else None
    dbg_pool = nc.dram_tensor("dbg_pool", [128, 2, 128], BF16, kind="ExternalOutput").ap() if DEBUG else None
    S = Sched(nc)
    _dout_n = [0]

    def dout_res():
        _dout_n[0] += 1
        return 'dram_out%d' % _dout_n[0]
    with contextlib.ExitStack() as es:
        def sb(name, shape, dt):
            return es.enter_context(nc.sbuf_tensor(name, list(shape), dt))

        x_sb = sb("x_sb", [128, 18, D], F32)
        hT = sb("hT", [128, 8, 9 * 128], BF16)
        x0_v = hT[:, 6:8, :].rearrange("p c n -> p (c n)").bitcast(F32)[:, 0:D]
        wbuf = sb("wbuf", [128, 20480], BF16)
        Mall = sb("Mall", [128, 12, 392], BF16)
        ident = sb("ident", [128, 128], BF16)
        identf = sb("identf", [128, 128], F32)
        bones = sb("bones", [128, 128], BF16)
        hmT = sb("hmT", [128, 8, 128], BF16)
        scr = sb("scr", [128, 2304], BF16)
        xs = scr[:, 0:1024]
        xs2 = scr[:, 1024:2048]
        sq = sb("sq", [128, 8, 128], BF16)
        rs = sb("rs", [128, 8, 128], F32)
        qnb = [sb("qn%d" % i, [128, 6, 128], BF16) for i in range(2)]
        kT = [sb("kT%d" % i, [128, 2, 128], BF16) for i in range(3)]
        kf = sb("kf", [128, 2, 128], F32)
        Vaug = [sb("Vaug%d" % i, [128, 4, 65], BF16) for i in range(3)]
        vf = sb("vf", [128, 256], F32)
        uextb = [sb("uext%d" % i, [128, 2, 144], F32) for i in range(2)]
        sA = sb("sA", [128, 768], F32)
        sBb = sb("sBb", [128, 768], F32)
        cdist = sA[:, 0:392]
        cvalid = sBb[:, 0:392]
        tmpM = rs[:].rearrange("p c n -> p (c n)")[:, 0:392]
        Wk = sb("Wk", [128, 2, 128], F32)
        bonesf = Wk[:, 0, :]
        pooledb = [sb("pooled%d" % i, [128, 2, 128], BF16) for i in range(2)]
        PTg = [sb("PTg%d" % i, [128, 768], BF16) for i in range(2)]
        PT = [PTg[i][:, 0:256].rearrange("p (c n) -> p c n", c=2) for i in range(2)]
        PTn = PTg[0][:, 256:384]
        mixtok = sb("mixtok", [128, 768], BF16)
        mixT = sb("mixT", [128, 8, 128], BF16)
        tab = sb("tab_s", [128, 2, 2, 128], F32)
        pwbd = sb("pwbd", [128, 2, 128], BF16)
        flag = sb("flag_s", [128, 1], F32)
        fence = sb("fence", [128, 1], F32)
        gmixT = sb("gmixT", [128, 2, 8], F32)
        gffnT = sb("gffnT", [128, 2, 8], F32)
        gq = sb("gq", [128, 2], F32)
        gk = sb("gk", [128, 2], F32)
        pscale = sb("pscale", [128, 2, 2], F32)
        esink = sb("esink", [128, 2, 12], F32)
        ss1 = sb("ss1", [128, 1], F32)
        rstd1 = sb("rstd1", [128, 1], F32)
        ss2 = sb("ss2", [128, 1], F32)
        rstd2 = sb("rstd2", [128, 1], F32)
        den = sb("den", [128, 12], F32)
        rden = sb("rden", [128, 12], F32)
        routerw = sb("routerw", [128, 8, NE], BF16)
        lg = sb("lg", [128, NE], F32)
        lg2 = sb("lg2", [128, NE], F32)
        msk = sb("msk", [128, NE], F32)
        m1 = sb("m1", [128, 1], F32)
        m2 = sb("m2", [128, 1], F32)
        nm1 = sb("nm1", [128, 1], F32)
        rr = sb("rr", [128, 1], F32)
        comb = sb("comb", [128, 19, NE], F32)
        ost = sb("ost", [128, 512], F32)
        outst = [ost[:, i * 256:(i + 1) * 256] for i in range(2)]
        sptok = ost[0:120, :].rearrange("p (h n) -> p h n", h=2)
        ktok = [sb("ktok%d" % i, [128, 256], BF16) for i in range(2)]
        kTs = sb("kTs", [128, 2, 16, 128], BF16)
        Vs = sb("Vs", [128, 16, 4, 65], BF16)
        PTz = sb("PTz", [128, 16, 128], BF16)
        us = sb("us", [128, 2, 16, 24], F32)
        sg = [scr[:, i * 384:(i + 1) * 384] for i in range(2)]
        aT = [scr[:, 768 + i * 768:768 + (i + 1) * 768].rearrange("p (g n) -> p g n", g=G) for i in range(2)]

        banks = [es.enter_context(nc.psum_tensor("bank%d" % i, [128, 512], F32)) for i in range(8)]

        Win = wbuf[:, 0:12288].rearrange("p (k n) -> p k n", k=8)
        Wout = wbuf[:, 12288:20480].rearrange("p (k n) -> p k n", k=8)
        SLOTSZ = 6144
        def slot_views(s):
            base = s * SLOTSZ
            wg = wbuf[:, base:base + 2048].rearrange("p (k n) -> p k n", k=8)
            wu = wbuf[:, base + 2048:base + 4096].rearrange("p (k n) -> p k n", k=8)
            wd = wbuf[:, base + 4096:base + 6144].rearrange("p (g n) -> p g n", g=G)
            return wg, wu, wd

        def xt(t):
            return x0_v if t == 0 else x_sb[:, t - 1, :]

        QKh = banks[0][:].rearrange("p (c n) -> p c n", c=4)
        SSh = banks[1][:].rearrange("p (c n) -> p c n", c=4)
        U_ps = banks[4][:, 0:256].rearrange("p (c n) -> p c n", c=2)
        V_ps = banks[4][:, 256:512]
        TR = banks[5][:].bitcast(BF16).rearrange("p (c n) -> p c n", c=8)
        TRF = banks[5][:]
        TR2 = banks[6][:].bitcast(BF16).rearrange("p (c n) -> p c n", c=8)
        TRF2 = banks[6][:]
        ST = [banks[6][:, 0:256].rearrange("p (c n) -> p c n", c=2), banks[7][:, 0:256].rearrange("p (c n) -> p c n", c=2)]
        def o_ps(h):
            return banks[2 + h // 6][:, (h % 6) * 65:(h % 6) * 65 + 65]
        sAv = sA[:, 0:288].rearrange("p (c n) -> p c n", c=2)
        sBv = sBb[:, 0:288].rearrange("p (c n) -> p c n", c=2)
        sAs = sA[:].rearrange("p (c b n) -> p c b n", c=2, b=16)
        sBs = sBb[:].rearrange("p (c b n) -> p c b n", c=2, b=16)

        def load_mixer_weights(l, skip=()):
            for k in range(8):
                if ('in', k) in skip:
                    continue
                S.dma('pool', lambda e, k=k: e.dma_start(out=Win[:, k, :], in_=w_in_d[l, k * 128:(k + 1) * 128, :]),
                      writes=['Win%d' % k], chan='w')
            for k in range(8):
                if ('out', k) in skip:
                    continue
                S.dma('pool', lambda e, k=k: e.dma_start(out=Wout[:, k, :], in_=w_out_d[l, k * 128:(k + 1) * 128, :]),
                      writes=['Wout%d' % k], chan='w')
            S.op('pool', lambda e: e.memset(pwbd[:], 0.0), writes=['pwbd'])
            for g in range(4):
                c, po = g // 2, (g % 2) * 64
                S.dma('pool', lambda e, g=g, c=c, po=po: e.dma_start(out=pwbd[po:po + 64, c, po:po + 64], in_=pool_w_d[l, g]),
                      reads=[], writes=['pwbd'], chan='w')

        load_mixer_weights(0)
        S.dma('sp', lambda e: e.dma_start(out=cdist, in_=cdist_d), writes=['cdist'])
        S.dma('sp', lambda e: e.dma_start(out=cvalid, in_=cvalid_d), writes=['cvalid'])
        S.dma('sp', lambda e: e.dma_start(out=tab[:], in_=tab_d), writes=['tab'])
        S.dma('sp', lambda e: e.dma_start(out=flag[:], in_=flag_d), writes=['flag'])
        for l in range(2):
            S.dma('sp', lambda e, l=l: e.dma_start(out=gmixT[:, l, :], in_=norm_mix_d[l].rearrange("(c p) -> p c", p=128)),
                  writes=['gmixT'])
            S.dma('sp', lambda e, l=l: e.dma_start(out=gffnT[:, l, :], in_=norm_ffn_d[l].rearrange("(c p) -> p c", p=128)),
                  writes=['gffnT'])
            for hh in range(2):
                S.dma('sp', lambda e, l=l, hh=hh: e.dma_start(out=gq[hh * 64:(hh + 1) * 64, l:l + 1],
                                                            in_=qn_d[l].rearrange("(p o) -> p o", o=1)), writes=['gq'])
                S.dma('sp', lambda e, l=l, hh=hh: e.dma_start(out=gk[hh * 64:(hh + 1) * 64, l:l + 1],
                                                            in_=kn_d[l].rearrange("(p o) -> p o", o=1)), writes=['gk'])
            S.dma('sp', lambda e, l=l: e.dma_start(out=pscale[:, l, :], in_=pool_scale_d[l].rearrange("(c p) -> p c", p=128)),
                  writes=['pscale'])
            S.dma('sp', lambda e, l=l: e.dma_start(out=esink[:, l, :], in_=sinks_d[l:l + 1, :].partition_broadcast(128)),
                  writes=['esink'])
        S.op('act', lambda e: e.activation(out=esink[:], in_=esink[:], func=AF.Exp), reads=['esink'], writes=['esink'])
        S.op('dve', lambda e: e.tensor_scalar(out=gq[:], in0=gq[:], scalar1=0.125, scalar2=None, op0=ALU.mult),
             reads=['gq'], writes=['gq'])
        S.op('pool', lambda e: e.memset(identf[:], 1.0), writes=['identf'])
        S.op('pool', lambda e: e.affine_select(out=identf[:], in_=identf[:], pattern=[[-1, 128]],
                                              compare_op=ALU.is_equal, fill=0.0, base=0, channel_multiplier=1),
             reads=['identf'], writes=['identf'])
        S.op('pool', lambda e: e.tensor_copy(out=ident[:], in_=identf[:]), reads=['identf'], writes=['ident'])
        S.op('pool', lambda e: e.memset(bonesf, 0.0), writes=['bonesf'])
        S.op('pool', lambda e: e.memset(bonesf[0:64, 0:64], 1.0 / 64), reads=['bonesf'], writes=['bonesf'])
        S.op('pool', lambda e: e.memset(bonesf[64:128, 64:128], 1.0 / 64), reads=['bonesf'], writes=['bonesf'])
        S.op('pool', lambda e: e.tensor_copy(out=bones[:], in_=bonesf), reads=['bonesf'], writes=['bones'])
        S.op('pool', lambda e: e.memset(PTz[:], 0.0), writes=['PTz'])
        for i in range(3):
            S.op('pool', lambda e, i=i: e.memset(Vaug[i][:], 1.0), writes=['Vaug%d' % i])
        S.op('pool', lambda e: e.memset(Vs[:], 1.0), writes=['Vs'])
        S.op('pool', lambda e: e.memset(us[:], 0.0), writes=['us'])
        for i in range(2):
            S.op('pool', lambda e, i=i: e.memset(uextb[i][:], 0.0), writes=['uext%d' % i])
        for h in range(12):
            S.op('act', lambda e, h=h: e.activation(out=tmpM, in_=cdist, func=AF.Exp, scale=-SLOPES[h]),
                 reads=['cdist'], writes=['tmpM'])
            S.op('dve', lambda e, h=h: e.tensor_tensor(out=Mall[:, h, :], in0=tmpM, in1=cvalid, op=ALU.mult),
                 reads=['tmpM', 'cvalid'], writes=['Mall'])
        for t in range(2):
            S.dma('sp', lambda e, t=t: e.dma_start(out=xt(t), in_=xin[t * 128:(t + 1) * 128, :]), writes=['x%d' % t], chan='x')
        S.dma('pool', lambda e: e.dma_start(out=routerw[:], in_=router_d[0].rearrange("(k p) n -> p k n", p=128)),
              writes=['routerw'], chan='w')

        def norm_T(t, gT_l, dst, dst_res, which):
            X = xt(t)
            xr = 'x%d' % t
            xsb, ssb, rsb = ((xs, ss1, rstd1), (xs2, ss2, rstd2))[which]
            xn, sn, rn = 'xs%d' % which, 'ss%d' % which, 'rstd%d' % which
            TRw, bk = ((TR, 5), (TR2, 6))[which]
            S.op('act', lambda e: e.activation(out=xsb, in_=X, func=AF.Square, accum_out=ssb[:]),
                 reads=[xr], writes=[xn, sn])
            S.op('act', lambda e: e.activation(out=rsb[:], in_=ssb[:], func=AF.Ln, scale=1.0 / D, bias=EPS),
                 reads=[sn], writes=[rn])
            S.op('act', lambda e: e.activation(out=rsb[:], in_=rsb[:], func=AF.Exp, scale=-0.5),
                 reads=[rn], writes=[rn])
            yield
            S.op('dve', lambda e: e.tensor_scalar(out=xsb, in0=X, scalar1=rsb[:, 0:1], scalar2=None, op0=ALU.mult),
                 reads=[xr, rn], writes=[xn])
            yield
            for c in range(8):
                S.op('pe', lambda e, c=c: e.transpose(out=TRw[:, c, :], in_=xsb[:, c * 128:(c + 1) * 128], identity=ident[:]),
                     reads=[xn, 'ident'], banks=[bk], inc=(c == 7))
            yield
            S.op('dve', lambda e: e.tensor_tensor(out=dst, in0=TRw, in1=gT_l.unsqueeze(2).broadcast_to([128, 8, 128]),
                                                 op=ALU.mult), reads=['gmixT', 'gffnT'], writes=[dst_res], banks=[bk])
            yield

        def project(l, t, r, halo, want_state):
            qn = qnb[PAR(t)]
            qres = 'qn%d' % PAR(t)
            yield from norm_T(t, gmixT[:, l, :], hmT[:], 'hmT', 0)
            for half in range(2):
                for cc in range(4):
                    c = half * 4 + cc
                    for k in range(8):
                        S.op('pe', lambda e, c=c, cc=cc, k=k: e.matmul(QKh[:, cc, :], lhsT=Win[:, k, 256 + c * 128:256 + (c + 1) * 128],
                                                                      rhs=hmT[:, k, :], start=(k == 0), stop=(k == 7)),
                             reads=['hmT', 'Win%d' % k], banks=[0], inc=(k == 7 and cc == 3))
                yield
                if half == 0:
                    for c in range(2):
                        for k in range(8):
                            S.op('pe', lambda e, c=c, k=k: e.matmul(U_ps[:, c, :], lhsT=Win[:, k, c * 128:(c + 1) * 128],
                                                                   rhs=hmT[:, k, :], start=(k == 0), stop=(k == 7)),
                                 reads=['hmT', 'Win%d' % k], banks=[4], inc=False)
                    for k in range(8):
                        S.op('pe', lambda e, k=k: e.matmul(V_ps, lhsT=hmT[:, k, :], rhs=Win[:, k, 1280:1536],
                                                          start=(k == 0), stop=(k == 7)),
                             reads=['hmT', 'Win%d' % k], banks=[4], inc=(k == 7))
                S.op('act', lambda e, half=half: e.activation(out=sq[:, 4 * half:4 * half + 4, :], in_=QKh, func=AF.Square),
                     writes=['sq%d' % half], banks=[0])
                yield
                for cc in range(4):
                    S.op('pe', lambda e, cc=cc, half=half: e.matmul(SSh[:, cc, :], lhsT=bones[:], rhs=sq[:, 4 * half + cc, :],
                                                                   start=True, stop=True),
                         reads=['sq%d' % half, 'bones'], banks=[1], inc=(cc == 3))
                yield
                S.op('act', lambda e, half=half: e.activation(out=rs[:, 4 * half:4 * half + 4, :], in_=SSh, func=AF.Ln, bias=EPS),
                     writes=['rs%d' % half], banks=[1])
                S.op('act', lambda e, half=half: e.activation(out=rs[:, 4 * half:4 * half + 4, :], in_=rs[:, 4 * half:4 * half + 4, :],
                                                             func=AF.Exp, scale=-0.5), reads=['rs%d' % half], writes=['rs%d' % half])
                yield
                if half == 0:
                    S.op('dve', lambda e: e.scalar_tensor_tensor(out=qn[:, 0:4, :], in0=QKh, scalar=gq[:, l:l + 1], in1=rs[:, 0:4, :],
                                                                op0=ALU.mult, op1=ALU.mult),
                         reads=['rs0', 'gq'], writes=[qres], banks=[0])
                else:
                    S.op('dve', lambda e: e.scalar_tensor_tensor(out=qn[:, 4:6, :], in0=QKh[:, 0:2, :], scalar=gq[:, l:l + 1],
                                                                in1=rs[:, 4:6, :], op0=ALU.mult, op1=ALU.mult),
                         reads=['rs1', 'gq'], writes=[qres], banks=[0])
                    S.op('dve', lambda e: e.scalar_tensor_tensor(out=kT[r][:], in0=QKh[:, 2:4, :], scalar=gk[:, l:l + 1],
                                                                in1=rs[:, 6:8, :], op0=ALU.mult, op1=ALU.mult),
                         reads=['rs1', 'gk'], writes=['kT%d' % r], banks=[0])
                    if want_state:
                        S.op('dve', lambda e: e.scalar_tensor_tensor(out=kf[:], in0=QKh[:, 2:4, :], scalar=gk[:, l:l + 1],
                                                                    in1=rs[:, 6:8, :], op0=ALU.mult, op1=ALU.mult),
                             reads=['rs1', 'gk'], writes=['kf'], banks=[0])
                yield
            vsrc = V_ps.rearrange("p (g d) -> p g d", g=4)
            if halo:
                S.op('act', lambda e: e.activation(out=Vaug[r][:, :, 64:65], in_=Vaug[r][:, :, 64:65], func=AF.Identity, scale=0.0,
                                                  bias=flag[:, 0:1]), reads=['flag'], writes=['Vaug%d' % r])
                S.op('act', lambda e: e.activation(out=Vaug[r][:, :, 0:64], in_=vsrc, func=AF.Copy, scale=flag[:, 0:1]),
                     reads=['flag'], writes=['Vaug%d' % r], banks=[4])
            else:
                S.op('act', lambda e: e.activation(out=Vaug[r][:, :, 64:65], in_=Vaug[r][:, :, 64:65], func=AF.Copy, scale=0.0,
                                                  bias=1.0), writes=['Vaug%d' % r])
                S.op('act', lambda e: e.activation(out=Vaug[r][:, :, 0:64], in_=vsrc, func=AF.Copy),
                     writes=['Vaug%d' % r], banks=[4])
            if want_state:
                S.op('dve', lambda e: e.tensor_copy(out=vf[:], in_=V_ps), writes=['vf'], banks=[4])
            if t != 18:
                S.op('act', lambda e: e.activation(out=uextb[t % 2][:, :, 16:144], in_=U_ps, func=AF.Copy),
                     writes=['uext%d' % (t % 2)], banks=[4])
            else:
                for c in range(2):
                    S.op('act', lambda e, c=c: e.activation(out=us[:, c, :, 16:24], in_=U_ps[:, c, :].rearrange("p (b i) -> p b i", b=16),
                                                           func=AF.Copy), writes=['us'], banks=[4])
            yield

        def pool_sums(src, A, B, n, tabv, sample, srcres='us'):
            def sl(v, a, b):
                return v[:, :, :, a:b] if sample else v[:, :, a:b]
            def pick(v, p0, c):
                o = 16
                return (v[p0:p0 + 64, c, :, o:o + 8] if sample else v[p0:p0 + 64, c, o:o + 128])
            def wk(p0, c):
                return (Wk[p0:p0 + 64, c, :].rearrange("p (b i) -> p b i", b=16) if sample else Wk[p0:p0 + 64, c, :])
            def tb(p0, c):
                return (tabv[p0:p0 + 64, c, :].rearrange("p (b i) -> p b i", b=16) if sample else tabv[p0:p0 + 64, c, :])
            srcr = 'us' if sample else srcres
            def shift_add(dst, srcv, lo, sh, rres, wres):
                if sample:
                    for c in range(2):
                        S.op('pool', lambda e, c=c: e.tensor_tensor(out=dst[:, c, :, lo:n], in0=srcv[:, c, :, lo:n],
                                                                   in1=srcv[:, c, :, lo - sh:n - sh], op=ALU.add),
                             reads=[rres], writes=[wres])
                else:
                    S.op('pool', lambda e: e.tensor_tensor(out=dst[:, :, lo:n], in0=srcv[:, :, lo:n], in1=srcv[:, :, lo - sh:n - sh],
                                                          op=ALU.add), reads=[rres], writes=[wres])
            shift_add(A, src, 1, 1, srcr, 'sA')
            S.op('pool', lambda e: e.tensor_tensor(out=wk(0, 0), in0=pick(A, 0, 0), in1=tb(0, 0), op=ALU.mult),
                 reads=['sA', 'tab'], writes=['Wk'])
            yield
            shift_add(B, A, 3, 2, 'sA', 'sB')
            S.op('pool', lambda e: e.tensor_tensor(out=wk(64, 0), in0=pick(B, 64, 0), in1=tb(64, 0), op=ALU.mult),
                 reads=['sB', 'tab'], writes=['Wk'])
            yield
            shift_add(A, B, 7, 4, 'sB', 'sA')
            S.op('pool', lambda e: e.tensor_tensor(out=wk(0, 1), in0=pick(A, 0, 1), in1=tb(0, 1), op=ALU.mult),
                 reads=['sA', 'tab'], writes=['Wk'])
            yield
            shift_add(B, A, 15, 8, 'sA', 'sB')
            S.op('pool', lambda e: e.tensor_tensor(out=wk(64, 1), in0=pick(B, 64, 1), in1=tb(64, 1), op=ALU.mult),
                 reads=['sB', 'tab'], writes=['Wk'])

        def pool_mix(l, t):
            sample = (t == 18)
            pooled = pooledb[PAR(t)]
            pres = 'pooled%d' % PAR(t)
            tabv = tab[:, :, 0, :] if t == 2 else tab[:, :, 1, :]
            if sample:
                yield from pool_sums(us[:], sAs, sBs, 24, tabv, True)
                for c in range(2):
                    S.op('pool', lambda e, c=c: e.tensor_tensor(out=pooled[:, c, :].rearrange("p (b i) -> p b i", b=16),
                                                               in0=Wk[:, c, :].rearrange("p (b i) -> p b i", b=16),
                                                               in1=us[:, c, :, 16:24], op=ALU.subtract),
                         reads=['Wk', 'us'], writes=[pres])
            else:
                ue, uep = uextb[t % 2], uextb[(t - 1) % 2]
                un, unp = 'uext%d' % (t % 2), 'uext%d' % ((t - 1) % 2)
                S.op('pool', lambda e: e.tensor_copy(out=ue[:, :, 0:16], in_=uep[:, :, 128:144]), reads=[unp, un], writes=[un])
                yield from pool_sums(ue[:], sAv, sBv, 144, tabv, False, un)
                S.op('pool', lambda e: e.tensor_tensor(out=pooled[:], in0=Wk[:], in1=ue[:, :, 16:144], op=ALU.subtract),
                     reads=['Wk', un], writes=[pres])
            yield

        def pool_project(l, t):
            pooled = pooledb[PAR(t)]
            pres = 'pooled%d' % PAR(t)
            PM = TRF2[:, 0:256].rearrange("p (c n) -> p c n", c=2)
            for c in range(2):
                S.op('pe', lambda e, c=c: e.matmul(PM[:, c, :], lhsT=pwbd[:, c, :], rhs=pooled[:, c, :], start=True, stop=True),
                     reads=[pres, 'pwbd'], banks=[6], inc=(c == 1))
            for c in range(2):
                S.op('act', lambda e, c=c: e.activation(out=mixT[:, c, :], in_=PM[:, c, :], func=AF.Copy, scale=pscale[:, l, c:c + 1]),
                     reads=['pscale'], writes=['mixT'], banks=[6])
            yield

        def fp32_T_out(src_fn, dst_dram_fn, rows, src_res, oi):
            for c in range(2):
                S.op('pe', lambda e, c=c: e.transpose(out=TRF[:, 256 + c * 128:256 + (c + 1) * 128], in_=src_fn(c), identity=identf[:]),
                     reads=[src_res, 'identf'], banks=[5], inc=(c == 1))
            S.op('dve', lambda e: e.tensor_copy(out=outst[oi], in_=TRF[:, 256:512]), writes=['outst%d' % oi], banks=[5])
            S.dma('sp', lambda e: e.dma_start(out=dst_dram_fn(), in_=outst[oi][rows[0]:rows[1], :]), reads=['outst%d' % oi],
                  writes=[dout_res()], chan='o')

        def attention_prompt(l, t, r):
            rp = (t - 1) % 3
            qn = qnb[t % 2]
            qres = 'qn%d' % (t % 2)
            STp = banks[6][:, 0:384].rearrange("p (h n) -> p h n", h=3)
            STc = banks[7][:, 0:384].rearrange("p (h n) -> p h n", h=3)
            def s_mm(g):
                po = (g % 2) * 64
                kc = g // 2
                qc0 = QPOS[3 * g][0]
                pb = g % 2
                P4 = PTg[pb][:].rearrange("p (c h n) -> p c h n", c=2, h=3)
                pres = 'PTg%d' % pb
                S.op('pe', lambda e: e.matmul(STp, lhsT=kT[rp][po:po + 64, kc, :], rhs=qn[po:po + 64, qc0:qc0 + 3, :],
                                              start=True, stop=True), reads=['kT%d' % rp, qres], banks=[6], inc=True)
                S.op('pe', lambda e: e.matmul(STc, lhsT=kT[r][po:po + 64, kc, :], rhs=qn[po:po + 64, qc0:qc0 + 3, :],
                                              start=True, stop=True), reads=['kT%d' % r, qres], banks=[7], inc=True)
                S.op('act', lambda e: e.activation(out=P4[:, 0], in_=STp, func=AF.Exp), writes=[pres + 'p'], banks=[6])
                S.op('act', lambda e: e.activation(out=P4[:, 1], in_=STc, func=AF.Exp), writes=[pres + 'c'], banks=[7])
                S.op('dve', lambda e: e.tensor_tensor(out=P4[:, 0], in0=P4[:, 0], in1=Mall[:, 3 * g:3 * g + 3, 0:128], op=ALU.mult),
                     reads=[pres + 'p', 'Mall'], writes=[pres + 'p'])
                S.op('dve', lambda e: e.tensor_tensor(out=P4[:, 1], in0=P4[:, 1], in1=Mall[:, 3 * g:3 * g + 3, 128:256], op=ALU.mult),
                     reads=[pres + 'c', 'Mall'], writes=[pres + 'c'])
            def pv_mm(g):
                pb = g % 2
                P4 = PTg[pb][:].rearrange("p (c h n) -> p c h n", c=2, h=3)
                pres = 'PTg%d' % pb
                for hh in range(3):
                    h = 3 * g + hh
                    S.op('pe', lambda e, h=h, hh=hh: e.matmul(o_ps(h), lhsT=P4[:, 0, hh, :], rhs=Vaug[rp][:, g, :], start=True, stop=False),
                         reads=[pres + 'p', 'Vaug%d' % rp], banks=[2 + h // 6], inc=False)
                    S.op('pe', lambda e, h=h, hh=hh: e.matmul(o_ps(h), lhsT=P4[:, 1, hh, :], rhs=Vaug[r][:, g, :], start=False, stop=True),
                         reads=[pres + 'c', 'Vaug%d' % r], banks=[2 + h // 6], inc=(hh == 2))
            s_mm(0)
            yield
            for g in range(4):
                if g + 1 < 4:
                    s_mm(g + 1)
                    yield
                pv_mm(g)
                yield

        def sample_kv_load(l, pair):
            for b in (2 * pair, 2 * pair + 1):
                S.dma('pool', lambda e, b=b: e.dma_start(out=Vs[:, b, :, 0:64], in_=sv_d[l, b].rearrange("k (g d) -> k g d", g=4)),
                      writes=['Vs'], chan='w')
                kb = ktok[b % 2]
                S.dma('pool', lambda e, b=b, kb=kb: e.dma_start(out=kb[:], in_=sk_d[l, b]), writes=['ktok%d' % (b % 2)], chan='w')

        def sample_k_transpose(l, pair):
            for b in (2 * pair, 2 * pair + 1):
                kb = ktok[b % 2]
                for c in range(2):
                    S.op('pe', lambda e, c=c, kb=kb: e.transpose(out=TR2[:, c, :], in_=kb[:, c * 128:(c + 1) * 128], identity=ident[:]),
                         reads=['ktok%d' % (b % 2), 'ident'], banks=[6], inc=(c == 1))
                S.op('act', lambda e, b=b: e.activation(out=kTs[:, :, b, :], in_=TR2[:, 0:2, :], func=AF.Copy),
                     writes=['kTs'], banks=[6])

        def attention_sample(l):
            r = RSLOT(18)
            qn = qnb[PAR(18)]
            qres = 'qn%d' % PAR(18)
            def head(h):
                g = h // 3
                qc, po = QPOS[h]
                kc = g // 2
                sbi = h % 2
                stb = ST[sbi]
                sflat = banks[6 + sbi][:, 0:256]
                for b in range(16):
                    S.op('pe', lambda e, b=b: e.matmul(sflat[:, b * 8:(b + 1) * 8], lhsT=kTs[po:po + 64, kc, b, :],
                                                      rhs=qn[po:po + 64, qc, b * 8:(b + 1) * 8], start=True, stop=True),
                         reads=['kTs', qres], banks=[6 + sbi], inc=False)
                S.op('pe', lambda e: e.matmul(sflat[:, 128:256], lhsT=kT[r][po:po + 64, kc, :], rhs=qn[po:po + 64, qc, :],
                                              start=True, stop=True), reads=['kT%d' % r, qres], banks=[6 + sbi], inc=True)
                S.op('act', lambda e: e.activation(out=PT[sbi], in_=stb, func=AF.Exp), writes=['PT%d' % sbi], banks=[6 + sbi])
                diag = bass.AP(PTz[:].tensor, PTz[:].offset, [list(PTz[:].ap[0]), [128 + 8, 16], [1, 8]])
                S.op('dve', lambda e, diag=diag: e.tensor_tensor(
                    out=diag, in0=PT[sbi][:, 0, :].rearrange("p (b i) -> p b i", b=16),
                    in1=Mall[:, h, 256:264].unsqueeze(1).broadcast_to([128, 16, 8]), op=ALU.mult),
                    reads=['PT%d' % sbi, 'Mall'], writes=['PTz'])
                S.op('dve', lambda e: e.tensor_tensor(out=PTn, in0=PT[sbi][:, 1, :], in1=Mall[:, h, 264:392], op=ALU.mult),
                     reads=['PT%d' % sbi, 'Mall'], writes=['PTn'])
                for b in range(16):
                    S.op('pe', lambda e, b=b: e.matmul(o_ps(h), lhsT=PTz[:, b, :], rhs=Vs[:, b, g, :], start=(b == 0), stop=False),
                         reads=['PTz', 'Vs'], banks=[2 + h // 6], inc=False)
                S.op('pe', lambda e: e.matmul(o_ps(h), lhsT=PTn, rhs=Vaug[r][:, g, :], start=False, stop=True),
                     reads=['PTn', 'Vaug%d' % r], banks=[2 + h // 6], inc=True)
            for h in range(12):
                head(h)
                yield

        def attn_finish(l, t):
            for b in range(2):
                ob = banks[2 + b][:, 0:390].rearrange("p (h d) -> p h d", h=6)
                S.op('dve', lambda e, b=b, ob=ob: e.tensor_tensor(out=den[:, 6 * b:6 * b + 6], in0=ob[:, :, 64],
                                                                 in1=esink[:, l, 6 * b:6 * b + 6], op=ALU.add),
                     reads=['esink'], writes=['den%d' % b], banks=[2 + b])
                S.op('dve', lambda e, b=b: e.reciprocal(out=rden[:, 6 * b:6 * b + 6], in_=den[:, 6 * b:6 * b + 6]),
                     reads=['den%d' % b], writes=['rden%d' % b])
                S.op('dve', lambda e, b=b, ob=ob: e.tensor_tensor(
                    out=mixtok[:, 384 * b:384 * b + 384].rearrange("p (h d) -> p h d", h=6), in0=ob[:, :, 0:64],
                    in1=rden[:, 6 * b:6 * b + 6].unsqueeze(2).broadcast_to([128, 6, 64]), op=ALU.mult),
                    reads=['rden%d' % b], writes=['mixtok'], banks=[2 + b])
            yield
            for c in range(6):
                S.op('pe', lambda e, c=c: e.transpose(out=TR2[:, c, :], in_=mixtok[:, c * 128:(c + 1) * 128], identity=ident[:]),
                     reads=['mixtok', 'ident'], banks=[6], inc=(c == 5))
            S.op('act', lambda e: e.activation(out=mixT[:, 2:8, :], in_=TR2[:, 0:6, :], func=AF.Copy), writes=['mixT'], banks=[6])
            yield

        def out_proj(l, t):
            for hf in range(2):
                for k in range(8):
                    S.op('pe', lambda e, hf=hf, k=k: e.matmul(banks[2 + hf][:], lhsT=mixT[:, k, :], rhs=Wout[:, k, hf * 512:(hf + 1) * 512],
                                                             start=(k == 0), stop=(k == 7)),
                         reads=['mixT', 'Wout%d' % k], banks=[2 + hf], inc=(k == 7))
            yield
            X = xt(t)
            for hf in range(2):
                S.op('dve', lambda e, hf=hf: e.tensor_tensor(out=X[:, hf * 512:(hf + 1) * 512], in0=banks[2 + hf][:],
                                                            in1=X[:, hf * 512:(hf + 1) * 512], op=ALU.add),
                     reads=['x%d' % t], writes=['x%d' % t], banks=[2 + hf])
            yield

        def router(t, col):
            LG = TRF2[:, 0:NE]
            for k in range(8):
                S.op('pe', lambda e, k=k: e.matmul(LG, lhsT=hT[:, k, col * 128:(col + 1) * 128], rhs=routerw[:, k, :],
                                                  start=(k == 0), stop=(k == 7)),
                     reads=['hT', 'routerw'], banks=[6], inc=(k == 7))
            S.op('dve', lambda e: e.tensor_copy(out=lg[:], in_=LG), writes=['lg'], banks=[6])
            S.op('dve', lambda e: e.reduce_max(out=m1[:], in_=lg[:], axis=mybir.AxisListType.X), reads=['lg'], writes=['m1'])
            S.op('dve', lambda e: e.tensor_scalar(out=msk[:], in0=lg[:], scalar1=m1[:, 0:1], scalar2=-1e30, op0=ALU.is_equal, op1=ALU.mult),
                 reads=['lg', 'm1'], writes=['msk'])
            S.op('dve', lambda e: e.tensor_tensor(out=lg2[:], in0=lg[:], in1=msk[:], op=ALU.add), reads=['lg', 'msk'], writes=['lg2'])
            S.op('dve', lambda e: e.reduce_max(out=m2[:], in_=lg2[:], axis=mybir.AxisListType.X), reads=['lg2'], writes=['m2'])
            S.op('dve', lambda e: e.tensor_scalar(out=msk[:], in0=lg[:], scalar1=m2[:, 0:1], scalar2=None, op0=ALU.is_ge),
                 reads=['lg', 'm2'], writes=['msk'])
            S.op('dve', lambda e: e.tensor_scalar(out=nm1[:], in0=m1[:], scalar1=-1.0, scalar2=None, op0=ALU.mult),
                 reads=['m1'], writes=['nm1'])
            S.op('act', lambda e: e.activation(out=lg2[:], in_=lg[:], func=AF.Exp, bias=nm1[:, 0:1]), reads=['lg', 'nm1'], writes=['lg2'])
            S.op('act', lambda e: e.activation(out=rr[:], in_=m2[:], func=AF.Exp, bias=nm1[:, 0:1]), reads=['m2', 'nm1'], writes=['rr'])
            S.op('dve', lambda e: e.tensor_scalar(out=rr[:], in0=rr[:], scalar1=1.0, scalar2=None, op0=ALU.add), reads=['rr'], writes=['rr'])
            S.op('dve', lambda e: e.reciprocal(out=rr[:], in_=rr[:]), reads=['rr'], writes=['rr'])
            S.op('dve', lambda e: e.tensor_tensor(out=lg2[:], in0=lg2[:], in1=msk[:], op=ALU.mult), reads=['lg2', 'msk'], writes=['lg2'])
            S.op('dve', lambda e: e.tensor_scalar(out=comb[:, t, :], in0=lg2[:], scalar1=rr[:, 0:1], scalar2=None, op0=ALU.mult),
                 reads=['lg2', 'rr'], writes=['comb'])
            yield

        def state_outputs(l, t):
            if t == 17:
                fp32_T_out(lambda c: kf[:, c, :], lambda: k_p_d[l], (0, 128), 'kf', 0)
                S.dma('sp', lambda e: e.dma_start(out=v_p_d[l], in_=vf[:]), reads=['vf'], writes=[dout_res()], chan='o')
                fp32_T_out(lambda c: uextb[1][:, c, 16:144], lambda: pool_p_d[l], (0, 128), 'uext1', 1)
            if t == 18:
                fp32_T_out(lambda c: kf[:, c, :], lambda: k_s_new_d[l], (0, 128), 'kf', 0)
                S.dma('sp', lambda e: e.dma_start(out=v_s_new_d[l], in_=vf[:]), reads=['vf'], writes=[dout_res()], chan='o')
                for c in range(2):
                    S.op('pool', lambda e, c=c: e.tensor_copy(out=Wk[:, c, :].rearrange("p (b i) -> p b i", b=16), in_=us[:, c, :, 16:24]),
                         reads=['us'], writes=['Wk'])
                fp32_T_out(lambda c: Wk[:, c, :], lambda: pool_s_new_d[l], (0, 128), 'Wk', 1)

        def stage1(l, t):
            r = RSLOT(t)
            halo = t in (0, 1)
            kv_only = (l == 0 and t == 0) or (l == 1 and t == 1)
            want_state = t in (17, 18)
            yield from project(l, t, r, halo, want_state)
            if want_state:
                state_outputs(l, t)
                yield
            if not kv_only:
                yield from pool_mix(l, t)
            yield

        def stage2(l, t, col):
            r = RSLOT(t)
            kv_only = (l == 0 and t == 0) or (l == 1 and t == 1)
            if kv_only:
                return
            if 10 <= t <= 18:
                if t >= 11:
                    sample_k_transpose(l, t - 11)
                if t <= 17:
                    sample_kv_load(l, t - 10)
                yield
            if t == 18:
                yield from attention_sample(l)
            else:
                yield from attention_prompt(l, t, r)
            yield from attn_finish(l, t)
            if DEBUG and l == 0 and t == 18:
                S.dma('sp', lambda e: e.dma_start(out=dbg_attn, in_=mixtok[:]), reads=['mixtok'], writes=[dout_res()], chan='o')
                S.dma('sp', lambda e: e.dma_start(out=dbg_pool, in_=mixT[:, 0:2, :]), reads=['mixT'], writes=[dout_res()], chan='o')
            yield from pool_project(l, t)
            yield from out_proj(l, t)
            yield from norm_T(t, gffnT[:, l, :], hT[:, :, col * 128:(col + 1) * 128], 'hT', 1)
            if l == 1:
                yield from router(t, col)

        def run_interleaved(gens):
            active = list(gens)
            while active:
                for g_ in list(active):
                    try:
                        next(g_)
                    except StopIteration:
                        active.remove(g_)

        def ffn_load(wg_d, wu_d, wd_d, grp, slot):
            wg, wu, wd = slot_views(slot)
            ng = len(grp)
            f0 = grp[0] * 128
            sres = 'slot%d' % slot
            S.dma('pool', lambda e: e.dma_start(
                out=wg[:, :, 0:ng * 128], in_=wg_d.rearrange("(k p) n -> p k n", p=128)[:, :, f0:f0 + ng * 128]),
                writes=[sres], chan='w')
            S.dma('pool', lambda e: e.dma_start(
                out=wu[:, :, 0:ng * 128], in_=wu_d.rearrange("(k p) n -> p k n", p=128)[:, :, f0:f0 + ng * 128]),
                writes=[sres + 'u'], chan='w')
            S.dma('pool', lambda e: e.dma_start(
                out=wd[:, 0:ng, :], in_=wd_d[f0:f0 + ng * 128, :].rearrange("(g p) n -> p g n", p=128)),
                writes=[sres + 'd'], chan='w')

        def ffn_prefetch(l):
            S.op('pool', lambda e: e.memset(fence[:], 0.0), writes=['fence'] + ['Win%d' % k for k in range(8)])
            if l == 0:
                wg_d, wu_d, wd_d = fg_d[0], fu_d[0], fd_d[0]
            else:
                wg_d, wu_d, wd_d = mg_d[0, 0], mu_d[0, 0], md_d[0, 0]
            for gi_ in range(2):
                ffn_load(wg_d, wu_d, wd_d, list(range(gi_ * G, gi_ * G + G)), gi_)
            return 2

        def mixer_prefetch(l, last_slot):
            free = [s_ for s_ in range(NSLOT) if s_ != last_slot]
            names = []
            for s_ in free:
                names += ['slot%d' % s_, 'slot%du' % s_, 'slot%dd' % s_]
            S.op('pool', lambda e: e.memset(fence[:], 0.0), writes=['fence'] + names)
            skip = set()
            for k in range(8):
                if (k // 4) in free:
                    S.dma('pool', lambda e, k=k: e.dma_start(out=Win[:, k, :], in_=w_in_d[l, k * 128:(k + 1) * 128, :]),
                          writes=['Win%d' % k], chan='w')
                    skip.add(('in', k))
            for k in range(8):
                if k >= 6 or 2 in free:
                    S.dma('pool', lambda e, k=k: e.dma_start(out=Wout[:, k, :], in_=w_out_d[l, k * 128:(k + 1) * 128, :]),
                          writes=['Wout%d' % k], chan='w')
                    skip.add(('out', k))
            return skip

        def ffn_segment(l, tiles, prefetched=0):
            ne = 1 if l == 0 else NE
            ncol = len(tiles)
            blocks = [list(range(i, min(i + 3, ncol))) for i in range(0, ncol, 3)]
            groups = [list(range(i, min(i + G, NFF))) for i in range(0, NFF, G)]
            gi = 0
            gubuf = 0
            pending = [None]
            for ex in range(ne):
                if l == 0:
                    wg_d, wu_d, wd_d = fg_d[0], fu_d[0], fd_d[0]
                else:
                    wg_d, wu_d, wd_d = mg_d[0, ex], mu_d[0, ex], md_d[0, ex]
                for grp in groups:
                    slot = gi % NSLOT
                    wg, wu, wd = slot_views(slot)
                    ng = len(grp)
                    sres = 'slot%d' % slot
                    if gi >= prefetched:
                        ffn_load(wg_d, wu_d, wd_d, grp, slot)
                    gi += 1
                    def do_block(blk, wg=wg, wu=wu, wd=wd, ng=ng, sres=sres, ex=ex):
                        nonlocal gubuf
                        nt = len(blk)
                        c0 = blk[0] * 128
                        ntok = nt * 128
                        ab = (gubuf // 2) % 2
                        for fi in range(ng):
                            gb = gubuf % 2
                            gubuf += 1
                            for k in range(8):
                                S.op('pe', lambda e, fi=fi, k=k: e.matmul(banks[6][:, 0:ntok], lhsT=wg[:, k, fi * 128:(fi + 1) * 128],
                                                                          rhs=hT[:, k, c0:c0 + ntok], start=(k == 0), stop=(k == 7)),
                                     reads=['hT', sres], banks=[6], inc=(k == 7))
                            for k in range(8):
                                S.op('pe', lambda e, fi=fi, k=k: e.matmul(banks[7][:, 0:ntok], lhsT=wu[:, k, fi * 128:(fi + 1) * 128],
                                                                          rhs=hT[:, k, c0:c0 + ntok], start=(k == 0), stop=(k == 7)),
                                     reads=['hT', sres + 'u'], banks=[7], inc=(k == 7))
                            S.op('act', lambda e, gb=gb: e.activation(out=sg[gb][:, 0:ntok], in_=banks[6][:, 0:ntok], func=AF.Silu),
                                 writes=['sg%d' % gb], banks=[6])
                            S.op('dve', lambda e, gb=gb, fi=fi, ab=ab: e.tensor_tensor(out=aT[ab][:, fi, 0:ntok], in0=banks[7][:, 0:ntok],
                                                                                      in1=sg[gb][:, 0:ntok], op=ALU.mult),
                                 reads=['sg%d' % gb], writes=['aT%d_%d' % (ab, fi)], banks=[7])
                        def down():
                            for j in range(nt):
                                for hf in range(2):
                                    for fi in range(ng):
                                        S.op('pe', lambda e, j=j, hf=hf, fi=fi, ab=ab: e.matmul(
                                            banks[2 * j + hf][:], lhsT=aT[ab][:, fi, j * 128:(j + 1) * 128], rhs=wd[:, fi, hf * 512:(hf + 1) * 512],
                                            start=(fi == 0), stop=(fi == ng - 1)),
                                            reads=['aT%d_%d' % (ab, fi), sres + 'd'], banks=[2 * j + hf], inc=(fi == ng - 1))
                                t = tiles[blk[j]]
                                X = xt(t)
                                for hf in range(2):
                                    if l == 0:
                                        S.op('dve', lambda e, j=j, hf=hf, X=X: e.tensor_tensor(
                                            out=X[:, hf * 512:(hf + 1) * 512], in0=banks[2 * j + hf][:], in1=X[:, hf * 512:(hf + 1) * 512], op=ALU.add),
                                            reads=['x%d' % t], writes=['x%d' % t], banks=[2 * j + hf])
                                    else:
                                        S.op('dve', lambda e, j=j, hf=hf, X=X, t=t, ex=ex: e.scalar_tensor_tensor(
                                            out=X[:, hf * 512:(hf + 1) * 512], in0=banks[2 * j + hf][:], scalar=comb[:, t, ex:ex + 1],
                                            in1=X[:, hf * 512:(hf + 1) * 512], op0=ALU.mult, op1=ALU.add),
                                            reads=['x%d' % t, 'comb'], writes=['x%d' % t], banks=[2 * j + hf])
                        if pending[0] is not None:
                            pending[0]()
                        pending[0] = down
                    for blk in blocks:
                        do_block(blk)
            if pending[0] is not None:
                pending[0]()
                pending[0] = None

        def y_out(ts):
            for t in ts:
                S.dma('sp', lambda e, t=t: e.dma_start(out=y_d[(t - 2) * 128:(t - 1) * 128, :], in_=xt(t)), reads=['x%d' % t],
                      writes=[dout_res()], chan='o')

        S.barrier()
        for t in range(2, NTILE):
            S.dma('sp', lambda e, t=t: e.dma_start(out=xt(t), in_=xin[t * 128:(t + 1) * 128, :]), writes=['x%d' % t], chan='x')

        def old_state_copies():
            for l in range(2):
                S.dma('sp', lambda e, l=l: e.dma_start(out=k_s_old_d[l], in_=sk_d[l, :, 8:128, :]), writes=[dout_res()], chan='o')
                S.dma('sp', lambda e, l=l: e.dma_start(out=v_s_old_d[l], in_=sv_d[l, :, 8:128, :]), writes=[dout_res()], chan='o')
                S.dma('sp', lambda e, l=l: e.dma_start(out=pool_s_old_d[l], in_=sp_d[l].rearrange("(b j) d -> b j d", j=15)[:, 8:15, :]),
                      writes=[dout_res()], chan='o')
        nxt_skip = set()
        for l in range(2):
            for si, seg in enumerate(SEGS):
                tiles = [t for t in seg if not (l == 1 and t == 0)]
                if not (l == 0 and si == 0):
                    load_mixer_weights(l, nxt_skip)
                if 18 in tiles:
                    for hb in range(2):
                        S.dma('sp', lambda e, hb=hb, l=l: e.dma_start(out=sptok[:, hb, :], in_=sp_d[l, hb * 120:(hb + 1) * 120, :]),
                              writes=['sptok'], chan='x')
                    for hb in range(2):
                        for c in range(2):
                            S.op('pe', lambda e, hb=hb, c=c: e.transpose(out=TRF[:, 0:120], in_=sptok[:, hb, c * 128:(c + 1) * 128],
                                                                        identity=identf[0:120, 0:120]),
                                 reads=['sptok', 'identf'], banks=[5])
                            S.op('dve', lambda e, hb=hb, c=c: e.tensor_copy(
                                out=us[:, c, hb * 8:(hb + 1) * 8, 1:16], in_=TRF[:, 0:120].rearrange("p (b j) -> p b j", j=15)),
                                writes=['us'], banks=[5])
                ffn_tiles = []
                cols = {}
                for t in tiles:
                    kv_only = (l == 0 and t == 0) or (l == 1 and t == 1)
                    cols[t] = len(ffn_tiles)
                    if not kv_only:
                        ffn_tiles.append(t)
                run_interleaved([stage1(l, tiles[0])])
                for i in range(1, len(tiles)):
                    run_interleaved([stage2(l, tiles[i - 1], cols[tiles[i - 1]]), stage1(l, tiles[i])])
                npre = ffn_prefetch(l)
                run_interleaved([stage2(l, tiles[-1], cols[tiles[-1]])])
                S.barrier()
                if DEBUG:
                    for t in [tt for tt in ffn_tiles if tt in (17, 18)]:
                        S.dma('sp', lambda e, t=t, l=l: e.dma_start(out=dbg_d[2 * l, (t - 17) * 128:(t - 16) * 128, :], in_=xt(t)),
                              reads=['x%d' % t], writes=[dout_res()], chan='o')
                if l == 0 and si == 0:
                    old_state_copies()
                ffn_segment(l, ffn_tiles, npre)
                nl, nsi = (l, si + 1) if si + 1 < len(SEGS) else (l + 1, 0)
                nxt_skip = set()
                if nl < 2:
                    nxt_skip = mixer_prefetch(nl, ((1 if l == 0 else NE) * ((NFF + G - 1) // G) - 1) % NSLOT)
                if l == 1 and si == len(SEGS) - 1:
                    y_out(ffn_tiles)
                S.barrier()
                if l == 1 and si < len(SEGS) - 1:
                    y_out(ffn_tiles)
                if DEBUG:
                    for t in [tt for tt in ffn_tiles if tt in (17, 18)]:
                        S.dma('sp', lambda e, t=t, l=l: e.dma_start(out=dbg_d[2 * l + 1, (t - 17) * 128:(t - 16) * 128, :], in_=xt(t)),
                              reads=['x%d' % t], writes=[dout_res()], chan='o')
        S.barrier()
        S.emit()
        S.close()
    return nc


_NC_CACHE = {}
_DBG = {}


def _consts():
    b = np.arange(128)[:, None].astype(np.float64)
    a = np.arange(128)[None, :].astype(np.float64)
    cd = np.zeros((128, 392), np.float32)
    cv = np.zeros((128, 392), np.float32)
    cd[:, 0:128] = a - b + 128
    cv[:, 0:128] = (a < b)
    cd[:, 128:256] = np.maximum(a - b, 0)
    cv[:, 128:256] = (a >= b)
    i8 = np.arange(8)[None, :].astype(np.float64)
    cd[:, 256:264] = 128 + i8 - b
    cv[:, 256:264] = (b > i8)
    kb, kj = np.arange(128)[:, None] // 8, np.arange(128)[:, None] % 8
    qb, qi = np.arange(128)[None, :] // 8, np.arange(128)[None, :] % 8
    cd[:, 264:392] = np.maximum(qi - kj, 0)
    cv[:, 264:392] = (kb == qb) & (kj <= qi)
    return cd, cv


def _tab(first_half):
    wins = [2, 4, 8, 16]
    tab = np.zeros((128, 2, 2, 128), np.float32)
    for c in range(2):
        for hh in range(2):
            w = wins[c * 2 + hh]
            tab[hh * 64:(hh + 1) * 64, c, 1, :] = 1.0 / w
            if first_half:
                cnt = np.minimum(w, np.arange(128) + 1).astype(np.float32)
                tab[hh * 64:(hh + 1) * 64, c, 0, :] = 1.0 / cnt
            else:
                tab[hh * 64:(hh + 1) * 64, c, 0, :] = 1.0 / w
    return tab


def kernel(x_prompt, x_sample, state_pool, state_win_k, state_win_v,
           norm_mix, w_in, q_norm, k_norm, attn_sinks, pool_w, pool_scale, w_out, norm_ffn,
           ffn_w_gate, ffn_w_up, ffn_w_down, moe_router, moe_w_gate, moe_w_up, moe_w_down):
    f = lambda a: np.ascontiguousarray(np.asarray(a, dtype=np.float32))
    x_prompt, x_sample = f(x_prompt), f(x_sample)
    state_pool, state_win_k, state_win_v = f(state_pool), f(state_win_k), f(state_win_v)
    w_in = f(w_in)
    qcols = []
    for (ha, hb) in QPAIR:
        qcols += list(range(256 + ha * 64, 256 + ha * 64 + 64)) + list(range(256 + hb * 64, 256 + hb * 64 + 64))
    cols = list(range(256)) + qcols + list(range(1024, 1536))
    w_in_p = np.ascontiguousarray(w_in[:, :, cols])
    cd, cv = _consts()
    shared = {
        "cdist": cd, "cvalid": cv,
        "norm_mix": f(norm_mix), "norm_ffn": f(norm_ffn), "w_in": w_in_p, "q_norm": f(q_norm), "k_norm": f(k_norm),
        "attn_sinks": f(attn_sinks), "pool_w": f(pool_w), "pool_scale": f(pool_scale), "w_out": f(w_out),
        "ffn_w_gate": f(ffn_w_gate), "ffn_w_up": f(ffn_w_up), "ffn_w_down": f(ffn_w_down),
        "moe_router": f(moe_router), "moe_w_gate": f(moe_w_gate), "moe_w_up": f(moe_w_up), "moe_w_down": f(moe_w_down),
    }
    in_maps = []
    for c in range(NCORE):
        seq, half = c // 2, c % 2
        main = x_prompt[seq, half * 2048:(half + 1) * 2048]
        halo = x_prompt[seq, 2048 - 256:2048] if half == 1 else np.zeros((256, D), np.float32)
        xs_ = x_sample[c * 16:(c + 1) * 16].reshape(128, D)
        m = dict(shared)
        m["xin"] = np.ascontiguousarray(np.concatenate([halo, main, xs_], 0))
        m["flag"] = np.full((128, 1), float(half), np.float32)
        m["tab"] = _tab(half == 0)
        m["st_pool"] = np.ascontiguousarray(state_pool[:, c * 16:(c + 1) * 16].reshape(2, 240, 256))
        m["st_k"] = np.ascontiguousarray(state_win_k[:, c * 16:(c + 1) * 16].reshape(2, 16, 128, 256))
        m["st_v"] = np.ascontiguousarray(state_win_v[:, c * 16:(c + 1) * 16].reshape(2, 16, 128, 256))
        in_maps.append(m)
    if "nc" not in _NC_CACHE:
        _NC_CACHE["nc"] = build()
    res = run_bass_kernel_spmd(_NC_CACHE["nc"], in_maps, core_ids=list(range(NCORE)))
    R = res.results
    if DEBUG:
        _DBG["dbg"] = [np.asarray(r["dbg"]) for r in R]
        _DBG["attn"] = [np.asarray(r["dbg_attn"]).astype(np.float32) for r in R]
        _DBG["pool"] = [np.asarray(r["dbg_pool"]).astype(np.float32) for r in R]
    y_prompt = np.zeros((4, 4096, D), np.float32)
    y_sample = np.zeros((128, 8, D), np.float32)
    pool_p = np.zeros((2, 4, 15, 256), np.float32)
    k_p = np.zeros((2, 4, 128, 4, 64), np.float32)
    v_p = np.zeros((2, 4, 128, 4, 64), np.float32)
    pool_s = np.zeros((2, 128, 15, 256), np.float32)
    k_s = np.zeros((2, 128, 128, 4, 64), np.float32)
    v_s = np.zeros((2, 128, 128, 4, 64), np.float32)
    for c in range(NCORE):
        seq, half = c // 2, c % 2
        r = R[c]
        y = np.asarray(r["y"])
        y_prompt[seq, half * 2048:(half + 1) * 2048] = y[0:2048]
        y_sample[c * 16:(c + 1) * 16] = y[2048:2176].reshape(16, 8, D)
        if half == 1:
            pool_p[:, seq] = np.asarray(r["pool_p"])[:, 113:128, :]
            k_p[:, seq] = np.asarray(r["k_p"]).reshape(2, 128, 4, 64)
            v_p[:, seq] = np.asarray(r["v_p"]).reshape(2, 128, 4, 64)
        sl = slice(c * 16, (c + 1) * 16)
        pool_s[:, sl, 0:7] = np.asarray(r["pool_s_old"])
        pool_s[:, sl, 7:15] = np.asarray(r["pool_s_new"]).reshape(2, 16, 8, 256)
        k_s[:, sl, 0:120] = np.asarray(r["k_s_old"]).reshape(2, 16, 120, 4, 64)
        k_s[:, sl, 120:128] = np.asarray(r["k_s_new"]).reshape(2, 16, 8, 4, 64)
        v_s[:, sl, 0:120] = np.asarray(r["v_s_old"]).reshape(2, 16, 120, 4, 64)
        v_s[:, sl, 120:128] = np.asarray(r["v_s_new"]).reshape(2, 16, 8, 4, 64)
    return (y_prompt, y_sample, pool_p, k_p, v_p, pool_s, k_s, v_s)
```

```python
import contextlib
import math
import numpy as np
import concourse.bass as bass
import concourse.mybir as mybir
from concourse.bass_utils import run_bass_kernel_spmd

F32 = mybir.dt.float32
BF16 = mybir.dt.bfloat16
AF = mybir.ActivationFunctionType
ALU = mybir.AluOpType

D = 1024
DFF = 2816
NFF = 22
NE = 8
EPS = 1e-6
NCORE = 8
import os
DEBUG = bool(os.environ.get("KDEBUG"))
NTILE = 19
SEGS = [[0, 1, 2, 3, 4, 5, 6, 7, 8, 9], [10, 11, 12, 13, 14, 15, 16, 17, 18]]


def PAR(t):
    return t % 2


def RSLOT(t):
    return t % 3
G = 2
NSLOT = 3
QPAIR = [(0, 3), (1, 4), (2, 5), (6, 9), (7, 10), (8, 11)]
QPOS = {}
for _j, (_a, _b) in enumerate(QPAIR):
    QPOS[_a] = (_j, 0)
    QPOS[_b] = (_j, 64)


def alibi_slopes(n):
    def p2(m):
        start = 2.0 ** (-8.0 / m)
        return [start ** (i + 1) for i in range(m)]
    if float(math.log2(n)).is_integer():
        return p2(n)
    c = 2 ** int(math.floor(math.log2(n)))
    return p2(c) + p2(2 * c)[0::2][: n - c]


SLOPES = [float(np.float32(s)) for s in alibi_slopes(12)]


class Sched:
    ENG = ('pe', 'act', 'dve', 'pool', 'sp')

    def __init__(self, nc, ndma=8):
        self.nc = nc
        self.prog = {e: [] for e in self.ENG}
        self.sems = {}
        self.cnt = {e: 0 for e in self.ENG}
        self.waited = {e: {} for e in self.ENG}
        self.res = {}
        self.bank = {}
        self.ndma = ndma
        self.dma_i = {}
        self.dma_val = {}
        self.stack = []

    def sem(self, key):
        if key not in self.sems:
            cm = self.nc.semaphore("s%d" % len(self.sems))
            self.sems[key] = cm.__enter__()
            self.stack.append(cm)
        return self.sems[key]

    def _need(self, eng, tok, waits, own_ok=True):
        if tok is None:
            return
        k, v = tok
        if k == eng and (eng == 'pe' or not own_ok):
            return
        if self.waited[eng].get(k, 0) >= v:
            return
        self.waited[eng][k] = v
        waits.append((k, v))

    def _deps(self, eng, reads, writes, banks, waits):
        for r in reads:
            st = self.res.setdefault(r, [None, []])
            self._need(eng, st[0], waits)
        for w in writes:
            st = self.res.setdefault(w, [None, []])
            self._need(eng, st[0], waits)
            for t in st[1]:
                self._need(eng, t, waits, own_ok=False)
        for b in banks:
            self._need(eng, self.bank.get(b), waits, own_ok=False)

    def _commit(self, tok, reads, writes, banks):
        for r in reads:
            self.res[r][1].append(tok)
        for w in writes:
            self.res[w] = [tok, []]
        for b in banks:
            self.bank[b] = tok

    def op(self, eng, fn, reads=(), writes=(), banks=(), inc=True):
        waits = []
        self._deps(eng, reads, writes, banks, waits)
        tok = (eng, self.cnt[eng] + 1)
        if inc:
            self.cnt[eng] += 1
        self.prog[eng].append((waits, fn, (eng, 1) if inc else None))
        self._commit(tok, reads, writes, banks)
        return tok

    def dma(self, q, fn, reads=(), writes=(), chan='d'):
        waits = []
        self._deps(q, reads, writes, (), waits)
        i = self.dma_i.get(chan, 0)
        self.dma_i[chan] = i + 1
        key = ('dma', chan, i % self.ndma)
        prev = self.dma_val.get(key, 0)
        if prev:
            self._need(q, (key, prev), waits)
        val = prev + 16
        self.dma_val[key] = val
        tok = (key, val)
        self.prog[q].append((waits, fn, (key, 16)))
        self._commit(tok, reads, writes, ())
        return tok

    def barrier(self):
        toks = [(e, self.cnt[e]) for e in self.ENG if self.cnt[e] > 0]
        toks += [(k, v) for k, v in self.dma_val.items()]
        for e in self.ENG:
            waits = []
            for t in toks:
                self._need(e, t, waits, own_ok=False)
            if waits:
                self.prog[e].append((waits, None, None))

    def emit(self):
        nc = self.nc
        for e in self.ENG:
            self.sem(e)
            for waits, fn, inc in self.prog[e]:
                for k, v in waits:
                    self.sem(k)
                if inc is not None:
                    self.sem(inc[0])
        engobj = {'pe': 'tensor', 'act': 'scalar', 'dve': 'vector', 'pool': 'gpsimd', 'sp': 'sync'}
        with nc.allow_non_contiguous_dma(reason="tiny strided parameter loads"), nc.Block() as block:
            for e in self.ENG:
                def body(engine, e=e):
                    for waits, fn, inc in self.prog[e]:
                        for k, v in waits:
                            engine.wait_ge(self.sems[k], v)
                        if fn is not None:
                            ins = fn(engine)
                            if inc is not None:
                                ins.then_inc(self.sems[inc[0]], inc[1])
                getattr(block, engobj[e])(body)

    def close(self):
        for cm in reversed(self.stack):
            cm.__exit__(None, None, None)


def build():
    nc = bass.Bass("TRN2", target_bir_lowering=False)

    def din(name, shape):
        return nc.dram_tensor(name, list(shape), F32, kind="ExternalInput").ap()

    def dout(name, shape):
        return nc.dram_tensor(name, list(shape), F32, kind="ExternalOutput").ap()

    xin = din("xin", [NTILE * 128, D])
    flag_d = din("flag", [128, 1])
    cdist_d = din("cdist", [128, 392])
    cvalid_d = din("cvalid", [128, 392])
    tab_d = din("tab", [128, 2, 2, 128])
    sp_d = din("st_pool", [2, 240, 256])
    sk_d = din("st_k", [2, 16, 128, 256])
    sv_d = din("st_v", [2, 16, 128, 256])
    norm_mix_d = din("norm_mix", [2, D])
    norm_ffn_d = din("norm_ffn", [2, D])
    w_in_d = din("w_in", [2, D, 1536])
    qn_d = din("q_norm", [2, 64])
    kn_d = din("k_norm", [2, 64])
    sinks_d = din("attn_sinks", [2, 12])
    pool_w_d = din("pool_w", [2, 4, 64, 64])
    pool_scale_d = din("pool_scale", [2, 256])
    w_out_d = din("w_out", [2, D, D])
    fg_d = din("ffn_w_gate", [1, D, DFF])
    fu_d = din("ffn_w_up", [1, D, DFF])
    fd_d = din("ffn_w_down", [1, DFF, D])
    router_d = din("moe_router", [1, D, NE])
    mg_d = din("moe_w_gate", [1, NE, D, DFF])
    mu_d = din("moe_w_up", [1, NE, D, DFF])
    md_d = din("moe_w_down", [1, NE, DFF, D])

    y_d = dout("y", [17 * 128, D])
    pool_p_d = dout("pool_p", [2, 128, 256])
    k_p_d = dout("k_p", [2, 128, 256])
    v_p_d = dout("v_p", [2, 128, 256])
    pool_s_new_d = dout("pool_s_new", [2, 128, 256])
    pool_s_old_d = dout("pool_s_old", [2, 16, 7, 256])
    k_s_new_d = dout("k_s_new", [2, 128, 256])
    k_s_old_d = dout("k_s_old", [2, 16, 120, 256])
    v_s_new_d = dout("v_s_new", [2, 128, 256])
    v_s_old_d = dout("v_s_old", [2, 16, 120, 256])

    dbg_d = dout("dbg", [4, 2 * 128, D]) if DEBUG else None
    dbg_attn = nc.dram_tensor("dbg_attn", [128, 768], BF16, kind="ExternalOutput").ap() if DEBUG else None
    dbg_pool = nc.dram_tensor("dbg_pool", [128, 2, 128], BF16, kind="ExternalOutput").ap() if DEBUG else None
    S = Sched(nc)
    _dout_n = [0]

    def dout_res():
        _dout_n[0] += 1
        return 'dram_out%d' % _dout_n[0]
    with contextlib.ExitStack() as es:
        def sb(name, shape, dt):
            return es.enter_context(nc.sbuf_tensor(name, list(shape), dt))

        x_sb = sb("x_sb", [128, 18, D], F32)
        hT = sb("hT", [128, 8, 9 * 128], BF16)
        x0_v = hT[:, 6:8, :].rearrange("p c n -> p (c n)").bitcast(F32)[:, 0:D]
        wbuf = sb("wbuf", [128, 20480], BF16)
        Mall = sb("Mall", [128, 12, 392], BF16)
        ident = sb("ident", [128, 128], BF16)
        identf = sb("identf", [128, 128], F32)
        bones = sb("bones", [128, 128], BF16)
        hmT = sb("hmT", [128, 8, 128], BF16)
        scr = sb("scr", [128, 2304], BF16)
        xs = scr[:, 0:1024]
        xs2 = scr[:, 1024:2048]
        sq = sb("sq", [128, 8, 128], BF16)
        rs = sb("rs", [128, 8, 128], F32)
        qnb = [sb("qn%d" % i, [128, 6, 128], BF16) for i in range(2)]
        kT = [sb("kT%d" % i, [128, 2, 128], BF16) for i in range(3)]
        kf = sb("kf", [128, 2, 128], F32)
        Vaug = [sb("Vaug%d" % i, [128, 4, 65], BF16) for i in range(3)]
        vf = sb("vf", [128, 256], F32)
        uextb = [sb("uext%d" % i, [128, 2, 144], F32) for i in range(2)]
        sA = sb("sA", [128, 768], F32)
        sBb = sb("sBb", [128, 768], F32)
        cdist = sA[:, 0:392]
        cvalid = sBb[:, 0:392]
        tmpM = rs[:].rearrange("p c n -> p (c n)")[:, 0:392]
        Wk = sb("Wk", [128, 2, 128], F32)
        bonesf = Wk[:, 0, :]
        pooledb = [sb("pooled%d" % i, [128, 2, 128], BF16) for i in range(2)]
        PTg = [sb("PTg%d" % i, [128, 768], BF16) for i in range(2)]
        PT = [PTg[i][:, 0:256].rearrange("p (c n) -> p c n", c=2) for i in range(2)]
        PTn = PTg[0][:, 256:384]
        mixtok = sb("mixtok", [128, 768], BF16)
        mixT = sb("mixT", [128, 8, 128], BF16)
        tab = sb("tab_s", [128, 2, 2, 128], F32)
        pwbd = sb("pwbd", [128, 2, 128], BF16)
        flag = sb("flag_s", [128, 1], F32)
        fence = sb("fence", [128, 1], F32)
        gmixT = sb("gmixT", [128, 2, 8], F32)
        gffnT = sb("gffnT", [128, 2, 8], F32)
        gq = sb("gq", [128, 2], F32)
        gk = sb("gk", [128, 2], F32)
        pscale = sb("pscale", [128, 2, 2], F32)
        esink = sb("esink", [128, 2, 12], F32)
        ss1 = sb("ss1", [128, 1], F32)
        rstd1 = sb("rstd1", [128, 1], F32)
        ss2 = sb("ss2", [128, 1], F32)
        rstd2 = sb("rstd2", [128, 1], F32)
        den = sb("den", [128, 12], F32)
        rden = sb("rden", [128, 12], F32)
        routerw = sb("routerw", [128, 8, NE], BF16)
        lg = sb("lg", [128, NE], F32)
        lg2 = sb("lg2", [128, NE], F32)
        msk = sb("msk", [128, NE], F32)
        m1 = sb("m1", [128, 1], F32)
        m2 = sb("m2", [128, 1], F32)
        nm1 = sb("nm1", [128, 1], F32)
        rr = sb("rr", [128, 1], F32)
        comb = sb("comb", [128, 19, NE], F32)
        ost = sb("ost", [128, 512], F32)
        outst = [ost[:, i * 256:(i + 1) * 256] for i in range(2)]
        sptok = ost[0:120, :].rearrange("p (h n) -> p h n", h=2)
        ktok = [sb("ktok%d" % i, [128, 256], BF16) for i in range(2)]
        kTs = sb("kTs", [128, 2, 16, 128], BF16)
        Vs = sb("Vs", [128, 16, 4, 65], BF16)
        PTz = sb("PTz", [128, 16, 128], BF16)
        us = sb("us", [128, 2, 16, 24], F32)
        sg = [scr[:, i * 384:(i + 1) * 384] for i in range(2)]
        aT = [scr[:, 768 + i * 768:768 + (i + 1) * 768].rearrange("p (g n) -> p g n", g=G) for i in range(2)]

        banks = [es.enter_context(nc.psum_tensor("bank%d" % i, [128, 512], F32)) for i in range(8)]

        Win = wbuf[:, 0:12288].rearrange("p (k n) -> p k n", k=8)
        Wout = wbuf[:, 12288:20480].rearrange("p (k n) -> p k n", k=8)
        SLOTSZ = 6144
        def slot_views(s):
            base = s * SLOTSZ
            wg = wbuf[:, base:base + 2048].rearrange("p (k n) -> p k n", k=8)
            wu = wbuf[:, base + 2048:base + 4096].rearrange("p (k n) -> p k n", k=8)
            wd = wbuf[:, base + 4096:base + 6144].rearrange("p (g n) -> p g n", g=G)
            return wg, wu, wd

        def xt(t):
            return x0_v if t == 0 else x_sb[:, t - 1, :]

        QKh = banks[0][:].rearrange("p (c n) -> p c n", c=4)
        SSh = banks[1][:].rearrange("p (c n) -> p c n", c=4)
        U_ps = banks[4][:, 0:256].rearrange("p (c n) -> p c n", c=2)
        V_ps = banks[4][:, 256:512]
        TR = banks[5][:].bitcast(BF16).rearrange("p (c n) -> p c n", c=8)
        TRF = banks[5][:]
        TR2 = banks[6][:].bitcast(BF16).rearrange("p (c n) -> p c n", c=8)
        TRF2 = banks[6][:]
        ST = [banks[6][:, 0:256].rearrange("p (c n) -> p c n", c=2), banks[7][:, 0:256].rearrange("p (c n) -> p c n", c=2)]
        def o_ps(h):
            return banks[2 + h // 6][:, (h % 6) * 65:(h % 6) * 65 + 65]
        sAv = sA[:, 0:288].rearrange("p (c n) -> p c n", c=2)
        sBv = sBb[:, 0:288].rearrange("p (c n) -> p c n", c=2)
        sAs = sA[:].rearrange("p (c b n) -> p c b n", c=2, b=16)
        sBs = sBb[:].rearrange("p (c b n) -> p c b n", c=2, b=16)

        def load_mixer_weights(l, skip=()):
            for k in range(8):
                if ('in', k) in skip:
                    continue
                S.dma('pool', lambda e, k=k: e.dma_start(out=Win[:, k, :], in_=w_in_d[l, k * 128:(k + 1) * 128, :]),
                      writes=['Win%d' % k], chan='w')
            for k in range(8):
                if ('out', k) in skip:
                    continue
                S.dma('pool', lambda e, k=k: e.dma_start(out=Wout[:, k, :], in_=w_out_d[l, k * 128:(k + 1) * 128, :]),
                      writes=['Wout%d' % k], chan='w')
            S.op('pool', lambda e: e.memset(pwbd[:], 0.0), writes=['pwbd'])
            for g in range(4):
                c, po = g // 2, (g % 2) * 64
                S.dma('pool', lambda e, g=g, c=c, po=po: e.dma_start(out=pwbd[po:po + 64, c, po:po + 64], in_=pool_w_d[l, g]),
                      reads=[], writes=['pwbd'], chan='w')

        load_mixer_weights(0)
        S.dma('sp', lambda e: e.dma_start(out=cdist, in_=cdist_d), writes=['cdist'])
        S.dma('sp', lambda e: e.dma_start(out=cvalid, in_=cvalid_d), writes=['cvalid'])
        S.dma('sp', lambda e: e.dma_start(out=tab[:], in_=tab_d), writes=['tab'])
        S.dma('sp', lambda e: e.dma_start(out=flag[:], in_=flag_d), writes=['flag'])
        for l in range(2):
            S.dma('sp', lambda e, l=l: e.dma_start(out=gmixT[:, l, :], in_=norm_mix_d[l].rearrange("(c p) -> p c", p=128)),
                  writes=['gmixT'])
            S.dma('sp', lambda e, l=l: e.dma_start(out=gffnT[:, l, :], in_=norm_ffn_d[l].rearrange("(c p) -> p c", p=128)),
                  writes=['gffnT'])
            for hh in range(2):
                S.dma('sp', lambda e, l=l, hh=hh: e.dma_start(out=gq[hh * 64:(hh + 1) * 64, l:l + 1],
                                                            in_=qn_d[l].rearrange("(p o) -> p o", o=1)), writes=['gq'])
                S.dma('sp', lambda e, l=l, hh=hh: e.dma_start(out=gk[hh * 64:(hh + 1) * 64, l:l + 1],
                                                            in_=kn_d[l].rearrange("(p o) -> p o", o=1)), writes=['gk'])
            S.dma('sp', lambda e, l=l: e.dma_start(out=pscale[:, l, :], in_=pool_scale_d[l].rearrange("(c p) -> p c", p=128)),
                  writes=['pscale'])
            S.dma('sp', lambda e, l=l: e.dma_start(out=esink[:, l, :], in_=sinks_d[l:l + 1, :].partition_broadcast(128)),
                  writes=['esink'])
        S.op('act', lambda e: e.activation(out=esink[:], in_=esink[:], func=AF.Exp), reads=['esink'], writes=['esink'])
        S.op('dve', lambda e: e.tensor_scalar(out=gq[:], in0=gq[:], scalar1=0.125, scalar2=None, op0=ALU.mult),
             reads=['gq'], writes=['gq'])
        S.op('pool', lambda e: e.memset(identf[:], 1.0), writes=['identf'])
        S.op('pool', lambda e: e.affine_select(out=identf[:], in_=identf[:], pattern=[[-1, 128]],
                                              compare_op=ALU.is_equal, fill=0.0, base=0, channel_multiplier=1),
             reads=['identf'], writes=['identf'])
        S.op('pool', lambda e: e.tensor_copy(out=ident[:], in_=identf[:]), reads=['identf'], writes=['ident'])
        S.op('pool', lambda e: e.memset(bonesf, 0.0), writes=['bonesf'])
        S.op('pool', lambda e: e.memset(bonesf[0:64, 0:64], 1.0 / 64), reads=['bonesf'], writes=['bonesf'])
        S.op('pool', lambda e: e.memset(bonesf[64:128, 64:128], 1.0 / 64), reads=['bonesf'], writes=['bonesf'])
        S.op('pool', lambda e: e.tensor_copy(out=bones[:], in_=bonesf), reads=['bonesf'], writes=['bones'])
        S.op('pool', lambda e: e.memset(PTz[:], 0.0), writes=['PTz'])
        for i in range(3):
            S.op('pool', lambda e, i=i: e.memset(Vaug[i][:], 1.0), writes=['Vaug%d' % i])
        S.op('pool', lambda e: e.memset(Vs[:], 1.0), writes=['Vs'])
        S.op('pool', lambda e: e.memset(us[:], 0.0), writes=['us'])
        for i in range(2):
            S.op('pool', lambda e, i=i: e.memset(uextb[i][:], 0.0), writes=['uext%d' % i])
        for h in range(12):
            S.op('act', lambda e, h=h: e.activation(out=tmpM, in_=cdist, func=AF.Exp, scale=-SLOPES[h]),
                 reads=['cdist'], writes=['tmpM'])
            S.op('dve', lambda e, h=h: e.tensor_tensor(out=Mall[:, h, :], in0=tmpM, in1=cvalid, op=ALU.mult),
                 reads=['tmpM', 'cvalid'], writes=['Mall'])
        for t in range(2):
            S.dma('sp', lambda e, t=t: e.dma_start(out=xt(t), in_=xin[t * 128:(t + 1) * 128, :]), writes=['x%d' % t], chan='x')
        S.dma('pool', lambda e: e.dma_start(out=routerw[:], in_=router_d[0].rearrange("(k p) n -> p k n", p=128)),
              writes=['routerw'], chan='w')

        def norm_T(t, gT_l, dst, dst_res, which):
            X = xt(t)
            xr = 'x%d' % t
            xsb, ssb, rsb = ((xs, ss1, rstd1), (xs2, ss2, rstd2))[which]
            xn, sn, rn = 'xs%d' % which, 'ss%d' % which, 'rstd%d' % which
            TRw, bk = ((TR, 5), (TR2, 6))[which]
            S.op('act', lambda e: e.activation(out=xsb, in_=X, func=AF.Square, accum_out=ssb[:]),
                 reads=[xr], writes=[xn, sn])
            S.op('act', lambda e: e.activation(out=rsb[:], in_=ssb[:], func=AF.Ln, scale=1.0 / D, bias=EPS),
                 reads=[sn], writes=[rn])
            S.op('act', lambda e: e.activation(out=rsb[:], in_=rsb[:], func=AF.Exp, scale=-0.5),
                 reads=[rn], writes=[rn])
            yield
            S.op('dve', lambda e: e.tensor_scalar(out=xsb, in0=X, scalar1=rsb[:, 0:1], scalar2=None, op0=ALU.mult),
                 reads=[xr, rn], writes=[xn])
            yield
            for c in range(8):
                S.op('pe', lambda e, c=c: e.transpose(out=TRw[:, c, :], in_=xsb[:, c * 128:(c + 1) * 128], identity=ident[:]),
                     reads=[xn, 'ident'], banks=[bk], inc=(c == 7))
            yield
            S.op('dve', lambda e: e.tensor_tensor(out=dst, in0=TRw, in1=gT_l.unsqueeze(2).broadcast_to([128, 8, 128]),
                                                 op=ALU.mult), reads=['gmixT', 'gffnT'], writes=[dst_res], banks=[bk])
            yield

        def project(l, t, r, halo, want_state):
            qn = qnb[PAR(t)]
            qres = 'qn%d' % PAR(t)
            yield from norm_T(t, gmixT[:, l, :], hmT[:], 'hmT', 0)
            for half in range(2):
                for cc in range(4):
                    c = half * 4 + cc
                    for k in range(8):
                        S.op('pe', lambda e, c=c, cc=cc, k=k: e.matmul(QKh[:, cc, :], lhsT=Win[:, k, 256 + c * 128:256 + (c + 1) * 128],
                                                                      rhs=hmT[:, k, :], start=(k == 0), stop=(k == 7)),
                             reads=['hmT', 'Win%d' % k], banks=[0], inc=(k == 7 and cc == 3))
                yield
                if half == 0:
                    for c in range(2):
                        for k in range(8):
                            S.op('pe', lambda e, c=c, k=k: e.matmul(U_ps[:, c, :], lhsT=Win[:, k, c * 128:(c + 1) * 128],
                                                                   rhs=hmT[:, k, :], start=(k == 0), stop=(k == 7)),
                                 reads=['hmT', 'Win%d' % k], banks=[4], inc=False)
                    for k in range(8):
                        S.op('pe', lambda e, k=k: e.matmul(V_ps, lhsT=hmT[:, k, :], rhs=Win[:, k, 1280:1536],
                                                          start=(k == 0), stop=(k == 7)),
                             reads=['hmT', 'Win%d' % k], banks=[4], inc=(k == 7))
                S.op('act', lambda e, half=half: e.activation(out=sq[:, 4 * half:4 * half + 4, :], in_=QKh, func=AF.Square),
                     writes=['sq%d' % half], banks=[0])
                yield
                for cc in range(4):
                    S.op('pe', lambda e, cc=cc, half=half: e.matmul(SSh[:, cc, :], lhsT=bones[:], rhs=sq[:, 4 * half + cc, :],
                                                                   start=True, stop=True),
                         reads=['sq%d' % half, 'bones'], banks=[1], inc=(cc == 3))
                yield
                S.op('act', lambda e, half=half: e.activation(out=rs[:, 4 * half:4 * half + 4, :], in_=SSh, func=AF.Ln, bias=EPS),
                     writes=['rs%d' % half], banks=[1])
                S.op('act', lambda e, half=half: e.activation(out=rs[:, 4 * half:4 * half + 4, :], in_=rs[:, 4 * half:4 * half + 4, :],
                                                             func=AF.Exp, scale=-0.5), reads=['rs%d' % half], writes=['rs%d' % half])
                yield
                if half == 0:
                    S.op('dve', lambda e: e.scalar_tensor_tensor(out=qn[:, 0:4, :], in0=QKh, scalar=gq[:, l:l + 1], in1=rs[:, 0:4, :],
                                                                op0=ALU.mult, op1=ALU.mult),
                         reads=['rs0', 'gq'], writes=[qres], banks=[0])
                else:
                    S.op('dve', lambda e: e.scalar_tensor_tensor(out=qn[:, 4:6, :], in0=QKh[:, 0:2, :], scalar=gq[:, l:l + 1],
                                                                in1=rs[:, 4:6, :], op0=ALU.mult, op1=ALU.mult),
                         reads=['rs1', 'gq'], writes=[qres], banks=[0])
                    S.op('dve', lambda e: e.scalar_tensor_tensor(out=kT[r][:], in0=QKh[:, 2:4, :], scalar=gk[:, l:l + 1],
                                                                in1=rs[:, 6:8, :], op0=ALU.mult, op1=ALU.mult),
                         reads=['rs1', 'gk'], writes=['kT%d' % r], banks=[0])
                    if want_state:
                        S.op('dve', lambda e: e.scalar_tensor_tensor(out=kf[:], in0=QKh[:, 2:4, :], scalar=gk[:, l:l + 1],
                                                                    in1=rs[:, 6:8, :], op0=ALU.mult, op1=ALU.mult),
                             reads=['rs1', 'gk'], writes=['kf'], banks=[0])
                yield
            vsrc = V_ps.rearrange("p (g d) -> p g d", g=4)
            if halo:
                S.op('act', lambda e: e.activation(out=Vaug[r][:, :, 64:65], in_=Vaug[r][:, :, 64:65], func=AF.Identity, scale=0.0,
                                                  bias=flag[:, 0:1]), reads=['flag'], writes=['Vaug%d' % r])
                S.op('act', lambda e: e.activation(out=Vaug[r][:, :, 0:64], in_=vsrc, func=AF.Copy, scale=flag[:, 0:1]),
                     reads=['flag'], writes=['Vaug%d' % r], banks=[4])
            else:
                S.op('act', lambda e: e.activation(out=Vaug[r][:, :, 64:65], in_=Vaug[r][:, :, 64:65], func=AF.Copy, scale=0.0,
                                                  bias=1.0), writes=['Vaug%d' % r])
                S.op('act', lambda e: e.activation(out=Vaug[r][:, :, 0:64], in_=vsrc, func=AF.Copy),
                     writes=['Vaug%d' % r], banks=[4])
            if want_state:
                S.op('dve', lambda e: e.tensor_copy(out=vf[:], in_=V_ps), writes=['vf'], banks=[4])
            if t != 18:
                S.op('act', lambda e: e.activation(out=uextb[t % 2][:, :, 16:144], in_=U_ps, func=AF.Copy),
                     writes=['uext%d' % (t % 2)], banks=[4])
            else:
                for c in range(2):
                    S.op('act', lambda e, c=c: e.activation(out=us[:, c, :, 16:24], in_=U_ps[:, c, :].rearrange("p (b i) -> p b i", b=16),
                                                           func=AF.Copy), writes=['us'], banks=[4])
            yield

        def pool_sums(src, A, B, n, tabv, sample, srcres='us'):
            def sl(v, a, b):
                return v[:, :, :, a:b] if sample else v[:, :, a:b]
            def pick(v, p0, c):
                o = 16
                return (v[p0:p0 + 64, c, :, o:o + 8] if sample else v[p0:p0 + 64, c, o:o + 128])
            def wk(p0, c):
                return (Wk[p0:p0 + 64, c, :].rearrange("p (b i) -> p b i", b=16) if sample else Wk[p0:p0 + 64, c, :])
            def tb(p0, c):
                return (tabv[p0:p0 + 64, c, :].rearrange("p (b i) -> p b i", b=16) if sample else tabv[p0:p0 + 64, c, :])
            srcr = 'us' if sample else srcres
            def shift_add(dst, srcv, lo, sh, rres, wres):
                if sample:
                    for c in range(2):
                        S.op('pool', lambda e, c=c: e.tensor_tensor(out=dst[:, c, :, lo:n], in0=srcv[:, c, :, lo:n],
                                                                   in1=srcv[:, c, :, lo - sh:n - sh], op=ALU.add),
                             reads=[rres], writes=[wres])
                else:
                    S.op('pool', lambda e: e.tensor_tensor(out=dst[:, :, lo:n], in0=srcv[:, :, lo:n], in1=srcv[:, :, lo - sh:n - sh],
                                                          op=ALU.add), reads=[rres], writes=[wres])
            shift_add(A, src, 1, 1, srcr, 'sA')
            S.op('pool', lambda e: e.tensor_tensor(out=wk(0, 0), in0=pick(A, 0, 0), in1=tb(0, 0), op=ALU.mult),
                 reads=['sA', 'tab'], writes=['Wk'])
            yield
            shift_add(B, A, 3, 2, 'sA', 'sB')
            S.op('pool', lambda e: e.tensor_tensor(out=wk(64, 0), in0=pick(B, 64, 0), in1=tb(64, 0), op=ALU.mult),
                 reads=['sB', 'tab'], writes=['Wk'])
            yield
            shift_add(A, B, 7, 4, 'sB', 'sA')
            S.op('pool', lambda e: e.tensor_tensor(out=wk(0, 1), in0=pick(A, 0, 1), in1=tb(0, 1), op=ALU.mult),
                 reads=['sA', 'tab'], writes=['Wk'])
            yield
            shift_add(B, A, 15, 8, 'sA', 'sB')
            S.op('pool', lambda e: e.tensor_tensor(out=wk(64, 1), in0=pick(B, 64, 1), in1=tb(64, 1), op=ALU.mult),
                 reads=['sB', 'tab'], writes=['Wk'])

        def pool_mix(l, t):
            sample = (t == 18)
            pooled = pooledb[PAR(t)]
            pres = 'pooled%d' % PAR(t)
            tabv = tab[:, :, 0, :] if t == 2 else tab[:, :, 1, :]
            if sample:
                yield from pool_sums(us[:], sAs, sBs, 24, tabv, True)
                for c in range(2):
                    S.op('pool', lambda e, c=c: e.tensor_tensor(out=pooled[:, c, :].rearrange("p (b i) -> p b i", b=16),
                                                               in0=Wk[:, c, :].rearrange("p (b i) -> p b i", b=16),
                                                               in1=us[:, c, :, 16:24], op=ALU.subtract),
                         reads=['Wk', 'us'], writes=[pres])
            else:
                ue, uep = uextb[t % 2], uextb[(t - 1) % 2]
                un, unp = 'uext%d' % (t % 2), 'uext%d' % ((t - 1) % 2)
                S.op('pool', lambda e: e.tensor_copy(out=ue[:, :, 0:16], in_=uep[:, :, 128:144]), reads=[unp, un], writes=[un])
                yield from pool_sums(ue[:], sAv, sBv, 144, tabv, False, un)
                S.op('pool', lambda e: e.tensor_tensor(out=pooled[:], in0=Wk[:], in1=ue[:, :, 16:144], op=ALU.subtract),
                     reads=['Wk', un], writes=[pres])
            yield

        def pool_project(l, t):
            pooled = pooledb[PAR(t)]
            pres = 'pooled%d' % PAR(t)
            PM = TRF2[:, 0:256].rearrange("p (c n) -> p c n", c=2)
            for c in range(2):
                S.op('pe', lambda e, c=c: e.matmul(PM[:, c, :], lhsT=pwbd[:, c, :], rhs=pooled[:, c, :], start=True, stop=True),
                     reads=[pres, 'pwbd'], banks=[6], inc=(c == 1))
            for c in range(2):
                S.op('act', lambda e, c=c: e.activation(out=mixT[:, c, :], in_=PM[:, c, :], func=AF.Copy, scale=pscale[:, l, c:c + 1]),
                     reads=['pscale'], writes=['mixT'], banks=[6])
            yield

        def fp32_T_out(src_fn, dst_dram_fn, rows, src_res, oi):
            for c in range(2):
                S.op('pe', lambda e, c=c: e.transpose(out=TRF[:, 256 + c * 128:256 + (c + 1) * 128], in_=src_fn(c), identity=identf[:]),
                     reads=[src_res, 'identf'], banks=[5], inc=(c == 1))
            S.op('dve', lambda e: e.tensor_copy(out=outst[oi], in_=TRF[:, 256:512]), writes=['outst%d' % oi], banks=[5])
            S.dma('sp', lambda e: e.dma_start(out=dst_dram_fn(), in_=outst[oi][rows[0]:rows[1], :]), reads=['outst%d' % oi],
                  writes=[dout_res()], chan='o')

        def attention_prompt(l, t, r):
            rp = (t - 1) % 3
            qn = qnb[t % 2]
            qres = 'qn%d' % (t % 2)
            STp = banks[6][:, 0:384].rearrange("p (h n) -> p h n", h=3)
            STc = banks[7][:, 0:384].rearrange("p (h n) -> p h n", h=3)
            def s_mm(g):
                po = (g % 2) * 64
                kc = g // 2
                qc0 = QPOS[3 * g][0]
                pb = g % 2
                P4 = PTg[pb][:].rearrange("p (c h n) -> p c h n", c=2, h=3)
                pres = 'PTg%d' % pb
                S.op('pe', lambda e: e.matmul(STp, lhsT=kT[rp][po:po + 64, kc, :], rhs=qn[po:po + 64, qc0:qc0 + 3, :],
                                              start=True, stop=True), reads=['kT%d' % rp, qres], banks=[6], inc=True)
                S.op('pe', lambda e: e.matmul(STc, lhsT=kT[r][po:po + 64, kc, :], rhs=qn[po:po + 64, qc0:qc0 + 3, :],
                                              start=True, stop=True), reads=['kT%d' % r, qres], banks=[7], inc=True)
                S.op('act', lambda e: e.activation(out=P4[:, 0], in_=STp, func=AF.Exp), writes=[pres + 'p'], banks=[6])
                S.op('act', lambda e: e.activation(out=P4[:, 1], in_=STc, func=AF.Exp), writes=[pres + 'c'], banks=[7])
                S.op('dve', lambda e: e.tensor_tensor(out=P4[:, 0], in0=P4[:, 0], in1=Mall[:, 3 * g:3 * g + 3, 0:128], op=ALU.mult),
                     reads=[pres + 'p', 'Mall'], writes=[pres + 'p'])
                S.op('dve', lambda e: e.tensor_tensor(out=P4[:, 1], in0=P4[:, 1], in1=Mall[:, 3 * g:3 * g + 3, 128:256], op=ALU.mult),
                     reads=[pres + 'c', 'Mall'], writes=[pres + 'c'])
            def pv_mm(g):
                pb = g % 2
                P4 = PTg[pb][:].rearrange("p (c h n) -> p c h n", c=2, h=3)
                pres = 'PTg%d' % pb
                for hh in range(3):
                    h = 3 * g + hh
                    S.op('pe', lambda e, h=h, hh=hh: e.matmul(o_ps(h), lhsT=P4[:, 0, hh, :], rhs=Vaug[rp][:, g, :], start=True, stop=False),
                         reads=[pres + 'p', 'Vaug%d' % rp], banks=[2 + h // 6], inc=False)
                    S.op('pe', lambda e, h=h, hh=hh: e.matmul(o_ps(h), lhsT=P4[:, 1, hh, :], rhs=Vaug[r][:, g, :], start=False, stop=True),
                         reads=[pres + 'c', 'Vaug%d' % r], banks=[2 + h // 6], inc=(hh == 2))
            s_mm(0)
            yield
            for g in range(4):
                if g + 1 < 4:
                    s_mm(g + 1)
                    yield
                pv_mm(g)
                yield

        def sample_kv_load(l, pair):
            for b in (2 * pair, 2 * pair + 1):
                S.dma('pool', lambda e, b=b: e.dma_start(out=Vs[:, b, :, 0:64], in_=sv_d[l, b].rearrange("k (g d) -> k g d", g=4)),
                      writes=['Vs'], chan='w')
                kb = ktok[b % 2]
                S.dma('pool', lambda e, b=b, kb=kb: e.dma_start(out=kb[:], in_=sk_d[l, b]), writes=['ktok%d' % (b % 2)], chan='w')

        def sample_k_transpose(l, pair):
            for b in (2 * pair, 2 * pair + 1):
                kb = ktok[b % 2]
                for c in range(2):
                    S.op('pe', lambda e, c=c, kb=kb: e.transpose(out=TR2[:, c, :], in_=kb[:, c * 128:(c + 1) * 128], identity=ident[:]),
                         reads=['ktok%d' % (b % 2), 'ident'], banks=[6], inc=(c == 1))
                S.op('act', lambda e, b=b: e.activation(out=kTs[:, :, b, :], in_=TR2[:, 0:2, :], func=AF.Copy),
                     writes=['kTs'], banks=[6])

        def attention_sample(l):
            r = RSLOT(18)
            qn = qnb[PAR(18)]
            qres = 'qn%d' % PAR(18)
            def head(h):
                g = h // 3
                qc, po = QPOS[h]
                kc = g // 2
                sbi = h % 2
                stb = ST[sbi]
                sflat = banks[6 + sbi][:, 0:256]
                for b in range(16):
                    S.op('pe', lambda e, b=b: e.matmul(sflat[:, b * 8:(b + 1) * 8], lhsT=kTs[po:po + 64, kc, b, :],
                                                      rhs=qn[po:po + 64, qc, b * 8:(b + 1) * 8], start=True, stop=True),
                         reads=['kTs', qres], banks=[6 + sbi], inc=False)
                S.op('pe', lambda e: e.matmul(sflat[:, 128:256], lhsT=kT[r][po:po + 64, kc, :], rhs=qn[po:po + 64, qc, :],
                                              start=True, stop=True), reads=['kT%d' % r, qres], banks=[6 + sbi], inc=True)
                S.op('act', lambda e: e.activation(out=PT[sbi], in_=stb, func=AF.Exp), writes=['PT%d' % sbi], banks=[6 + sbi])
                diag = bass.AP(PTz[:].tensor, PTz[:].offset, [list(PTz[:].ap[0]), [128 + 8, 16], [1, 8]])
                S.op('dve', lambda e, diag=diag: e.tensor_tensor(
                    out=diag, in0=PT[sbi][:, 0, :].rearrange("p (b i) -> p b i", b=16),
                    in1=Mall[:, h, 256:264].unsqueeze(1).broadcast_to([128, 16, 8]), op=ALU.mult),
                    reads=['PT%d' % sbi, 'Mall'], writes=['PTz'])
                S.op('dve', lambda e: e.tensor_tensor(out=PTn, in0=PT[sbi][:, 1, :], in1=Mall[:, h, 264:392], op=ALU.mult),
                     reads=['PT%d' % sbi, 'Mall'], writes=['PTn'])
                for b in range(16):
                    S.op('pe', lambda e, b=b: e.matmul(o_ps(h), lhsT=PTz[:, b, :], rhs=Vs[:, b, g, :], start=(b == 0), stop=False),
                         reads=['PTz', 'Vs'], banks=[2 + h // 6], inc=False)
                S.op('pe', lambda e: e.matmul(o_ps(h), lhsT=PTn, rhs=Vaug[r][:, g, :], start=False, stop=True),
                     reads=['PTn', 'Vaug%d' % r], banks=[2 + h // 6], inc=True)
            for h in range(12):
                head(h)
                yield

        def attn_finish(l, t):
            for b in range(2):
                ob = banks[2 + b][:, 0:390].rearrange("p (h d) -> p h d", h=6)
                S.op('dve', lambda e, b=b, ob=ob: e.tensor_tensor(out=den[:, 6 * b:6 * b + 6], in0=ob[:, :, 64],
                                                                 in1=esink[:, l, 6 * b:6 * b + 6], op=ALU.add),
                     reads=['esink'], writes=['den%d' % b], banks=[2 + b])
                S.op('dve', lambda e, b=b: e.reciprocal(out=rden[:, 6 * b:6 * b + 6], in_=den[:, 6 * b:6 * b + 6]),
                     reads=['den%d' % b], writes=['rden%d' % b])
                S.op('dve', lambda e, b=b, ob=ob: e.tensor_tensor(
                    out=mixtok[:, 384 * b:384 * b + 384].rearrange("p (h d) -> p h d", h=6), in0=ob[:, :, 0:64],
                    in1=rden[:, 6 * b:6 * b + 6].unsqueeze(2).broadcast_to([128, 6, 64]), op=ALU.mult),
                    reads=['rden%d' % b], writes=['mixtok'], banks=[2 + b])
            yield
            for c in range(6):
                S.op('pe', lambda e, c=c: e.transpose(out=TR2[:, c, :], in_=mixtok[:, c * 128:(c + 1) * 128], identity=ident[:]),
                     reads=['mixtok', 'ident'], banks=[6], inc=(c == 5))
            S.op('act', lambda e: e.activation(out=mixT[:, 2:8, :], in_=TR2[:, 0:6, :], func=AF.Copy), writes=['mixT'], banks=[6])
            yield

        def out_proj(l, t):
            for hf in range(2):
                for k in range(8):
                    S.op('pe', lambda e, hf=hf, k=k: e.matmul(banks[2 + hf][:], lhsT=mixT[:, k, :], rhs=Wout[:, k, hf * 512:(hf + 1) * 512],
                                                             start=(k == 0), stop=(k == 7)),
                         reads=['mixT', 'Wout%d' % k], banks=[2 + hf], inc=(k == 7))
            yield
            X = xt(t)
            for hf in range(2):
                S.op('dve', lambda e, hf=hf: e.tensor_tensor(out=X[:, hf * 512:(hf + 1) * 512], in0=banks[2 + hf][:],
                                                            in1=X[:, hf * 512:(hf + 1) * 512], op=ALU.add),
                     reads=['x%d' % t], writes=['x%d' % t], banks=[2 + hf])
            yield

        def router(t, col):
            LG = TRF2[:, 0:NE]
            for k in range(8):
                S.op('pe', lambda e, k=k: e.matmul(LG, lhsT=hT[:, k, col * 128:(col + 1) * 128], rhs=routerw[:, k, :],
                                                  start=(k == 0), stop=(k == 7)),
                     reads=['hT', 'routerw'], banks=[6], inc=(k == 7))
            S.op('dve', lambda e: e.tensor_copy(out=lg[:], in_=LG), writes=['lg'], banks=[6])
            S.op('dve', lambda e: e.reduce_max(out=m1[:], in_=lg[:], axis=mybir.AxisListType.X), reads=['lg'], writes=['m1'])
            S.op('dve', lambda e: e.tensor_scalar(out=msk[:], in0=lg[:], scalar1=m1[:, 0:1], scalar2=-1e30, op0=ALU.is_equal, op1=ALU.mult),
                 reads=['lg', 'm1'], writes=['msk'])
            S.op('dve', lambda e: e.tensor_tensor(out=lg2[:], in0=lg[:], in1=msk[:], op=ALU.add), reads=['lg', 'msk'], writes=['lg2'])
            S.op('dve', lambda e: e.reduce_max(out=m2[:], in_=lg2[:], axis=mybir.AxisListType.X), reads=['lg2'], writes=['m2'])
            S.op('dve', lambda e: e.tensor_scalar(out=msk[:], in0=lg[:], scalar1=m2[:, 0:1], scalar2=None, op0=ALU.is_ge),
                 reads=['lg', 'm2'], writes=['msk'])
            S.op('dve', lambda e: e.tensor_scalar(out=nm1[:], in0=m1[:], scalar1=-1.0, scalar2=None, op0=ALU.mult),
                 reads=['m1'], writes=['nm1'])
            S.op('act', lambda e: e.activation(out=lg2[:], in_=lg[:], func=AF.Exp, bias=nm1[:, 0:1]), reads=['lg', 'nm1'], writes=['lg2'])
            S.op('act', lambda e: e.activation(out=rr[:], in_=m2[:], func=AF.Exp, bias=nm1[:, 0:1]), reads=['m2', 'nm1'], writes=['rr'])
            S.op('dve', lambda e: e.tensor_scalar(out=rr[:], in0=rr[:], scalar1=1.0, scalar2=None, op0=ALU.add), reads=['rr'], writes=['rr'])
            S.op('dve', lambda e: e.reciprocal(out=rr[:], in_=rr[:]), reads=['rr'], writes=['rr'])
            S.op('dve', lambda e: e.tensor_tensor(out=lg2[:], in0=lg2[:], in1=msk[:], op=ALU.mult), reads=['lg2', 'msk'], writes=['lg2'])
            S.op('dve', lambda e: e.tensor_scalar(out=comb[:, t, :], in0=lg2[:], scalar1=rr[:, 0:1], scalar2=None, op0=ALU.mult),
                 reads=['lg2', 'rr'], writes=['comb'])
            yield

        def state_outputs(l, t):
            if t == 17:
                fp32_T_out(lambda c: kf[:, c, :], lambda: k_p_d[l], (0, 128), 'kf', 0)
                S.dma('sp', lambda e: e.dma_start(out=v_p_d[l], in_=vf[:]), reads=['vf'], writes=[dout_res()], chan='o')
                fp32_T_out(lambda c: uextb[1][:, c, 16:144], lambda: pool_p_d[l], (0, 128), 'uext1', 1)
            if t == 18:
                fp32_T_out(lambda c: kf[:, c, :], lambda: k_s_new_d[l], (0, 128), 'kf', 0)
                S.dma('sp', lambda e: e.dma_start(out=v_s_new_d[l], in_=vf[:]), reads=['vf'], writes=[dout_res()], chan='o')
                for c in range(2):
                    S.op('pool', lambda e, c=c: e.tensor_copy(out=Wk[:, c, :].rearrange("p (b i) -> p b i", b=16), in_=us[:, c, :, 16:24]),
                         reads=['us'], writes=['Wk'])
                fp32_T_out(lambda c: Wk[:, c, :], lambda: pool_s_new_d[l], (0, 128), 'Wk', 1)

        def stage1(l, t):
            r = RSLOT(t)
            halo = t in (0, 1)
            kv_only = (l == 0 and t == 0) or (l == 1 and t == 1)
            want_state = t in (17, 18)
            yield from project(l, t, r, halo, want_state)
            if want_state:
                state_outputs(l, t)
                yield
            if not kv_only:
                yield from pool_mix(l, t)
            yield

        def stage2(l, t, col):
            r = RSLOT(t)
            kv_only = (l == 0 and t == 0) or (l == 1 and t == 1)
            if kv_only:
                return
            if 10 <= t <= 18:
                if t >= 11:
                    sample_k_transpose(l, t - 11)
                if t <= 17:
                    sample_kv_load(l, t - 10)
                yield
            if t == 18:
                yield from attention_sample(l)
            else:
                yield from attention_prompt(l, t, r)
            yield from attn_finish(l, t)
            if DEBUG and l == 0 and t == 18:
                S.dma('sp', lambda e: e.dma_start(out=dbg_attn, in_=mixtok[:]), reads=['mixtok'], writes=[dout_res()], chan='o')
                S.dma('sp', lambda e: e.dma_start(out=dbg_pool, in_=mixT[:, 0:2, :]), reads=['mixT'], writes=[dout_res()], chan='o')
            yield from pool_project(l, t)
            yield from out_proj(l, t)
            yield from norm_T(t, gffnT[:, l, :], hT[:, :, col * 128:(col + 1) * 128], 'hT', 1)
            if l == 1:
                yield from router(t, col)

        def run_interleaved(gens):
            active = list(gens)
            while active:
                for g_ in list(active):
                    try:
                        next(g_)
                    except StopIteration:
                        active.remove(g_)

        def ffn_load(wg_d, wu_d, wd_d, grp, slot):
            wg, wu, wd = slot_views(slot)
            ng = len(grp)
            f0 = grp[0] * 128
            sres = 'slot%d' % slot
            S.dma('pool', lambda e: e.dma_start(
                out=wg[:, :, 0:ng * 128], in_=wg_d.rearrange("(k p) n -> p k n", p=128)[:, :, f0:f0 + ng * 128]),
                writes=[sres], chan='w')
            S.dma('pool', lambda e: e.dma_start(
                out=wu[:, :, 0:ng * 128], in_=wu_d.rearrange("(k p) n -> p k n", p=128)[:, :, f0:f0 + ng * 128]),
                writes=[sres + 'u'], chan='w')
            S.dma('pool', lambda e: e.dma_start(
                out=wd[:, 0:ng, :], in_=wd_d[f0:f0 + ng * 128, :].rearrange("(g p) n -> p g n", p=128)),
                writes=[sres + 'd'], chan='w')

        def ffn_prefetch(l):
            S.op('pool', lambda e: e.memset(fence[:], 0.0), writes=['fence'] + ['Win%d' % k for k in range(8)])
            if l == 0:
                wg_d, wu_d, wd_d = fg_d[0], fu_d[0], fd_d[0]
            else:
                wg_d, wu_d, wd_d = mg_d[0, 0], mu_d[0, 0], md_d[0, 0]
            for gi_ in range(2):
                ffn_load(wg_d, wu_d, wd_d, list(range(gi_ * G, gi_ * G + G)), gi_)
            return 2

        def mixer_prefetch(l, last_slot):
            free = [s_ for s_ in range(NSLOT) if s_ != last_slot]
            names = []
            for s_ in free:
                names += ['slot%d' % s_, 'slot%du' % s_, 'slot%dd' % s_]
            S.op('pool', lambda e: e.memset(fence[:], 0.0), writes=['fence'] + names)
            skip = set()
            for k in range(8):
                if (k // 4) in free:
                    S.dma('pool', lambda e, k=k: e.dma_start(out=Win[:, k, :], in_=w_in_d[l, k * 128:(k + 1) * 128, :]),
                          writes=['Win%d' % k], chan='w')
                    skip.add(('in', k))
            for k in range(8):
                if k >= 6 or 2 in free:
                    S.dma('pool', lambda e, k=k: e.dma_start(out=Wout[:, k, :], in_=w_out_d[l, k * 128:(k + 1) * 128, :]),
                          writes=['Wout%d' % k], chan='w')
                    skip.add(('out', k))
            return skip

        def ffn_segment(l, tiles, prefetched=0):
            ne = 1 if l == 0 else NE
            ncol = len(tiles)
            blocks = [list(range(i, min(i + 3, ncol))) for i in range(0, ncol, 3)]
            groups = [list(range(i, min(i + G, NFF))) for i in range(0, NFF, G)]
            gi = 0
            gubuf = 0
            pending = [None]
            for ex in range(ne):
                if l == 0:
                    wg_d, wu_d, wd_d = fg_d[0], fu_d[0], fd_d[0]
                else:
                    wg_d, wu_d, wd_d = mg_d[0, ex], mu_d[0, ex], md_d[0, ex]
                for grp in groups:
                    slot = gi % NSLOT
                    wg, wu, wd = slot_views(slot)
                    ng = len(grp)
                    sres = 'slot%d' % slot
                    if gi >= prefetched:
                        ffn_load(wg_d, wu_d, wd_d, grp, slot)
                    gi += 1
                    def do_block(blk, wg=wg, wu=wu, wd=wd, ng=ng, sres=sres, ex=ex):
                        nonlocal gubuf
                        nt = len(blk)
                        c0 = blk[0] * 128
                        ntok = nt * 128
                        ab = (gubuf // 2) % 2
                        for fi in range(ng):
                            gb = gubuf % 2
                            gubuf += 1
                            for k in range(8):
                                S.op('pe', lambda e, fi=fi, k=k: e.matmul(banks[6][:, 0:ntok], lhsT=wg[:, k, fi * 128:(fi + 1) * 128],
                                                                          rhs=hT[:, k, c0:c0 + ntok], start=(k == 0), stop=(k == 7)),
                                     reads=['hT', sres], banks=[6], inc=(k == 7))
                            for k in range(8):
                                S.op('pe', lambda e, fi=fi, k=k: e.matmul(banks[7][:, 0:ntok], lhsT=wu[:, k, fi * 128:(fi + 1) * 128],
                                                                          rhs=hT[:, k, c0:c0 + ntok], start=(k == 0), stop=(k == 7)),
                                     reads=['hT', sres + 'u'], banks=[7], inc=(k == 7))
                            S.op('act', lambda e, gb=gb: e.activation(out=sg[gb][:, 0:ntok], in_=banks[6][:, 0:ntok], func=AF.Silu),
                                 writes=['sg%d' % gb], banks=[6])
                            S.op('dve', lambda e, gb=gb, fi=fi, ab=ab: e.tensor_tensor(out=aT[ab][:, fi, 0:ntok], in0=banks[7][:, 0:ntok],
                                                                                      in1=sg[gb][:, 0:ntok], op=ALU.mult),
                                 reads=['sg%d' % gb], writes=['aT%d_%d' % (ab, fi)], banks=[7])
                        def down():
                            for j in range(nt):
                                for hf in range(2):
                                    for fi in range(ng):
                                        S.op('pe', lambda e, j=j, hf=hf, fi=fi, ab=ab: e.matmul(
                                            banks[2 * j + hf][:], lhsT=aT[ab][:, fi, j * 128:(j + 1) * 128], rhs=wd[:, fi, hf * 512:(hf + 1) * 512],
                                            start=(fi == 0), stop=(fi == ng - 1)),
                                            reads=['aT%d_%d' % (ab, fi), sres + 'd'], banks=[2 * j + hf], inc=(fi == ng - 1))
                                t = tiles[blk[j]]
                                X = xt(t)
                                for hf in range(2):
                                    if l == 0:
                                        S.op('dve', lambda e, j=j, hf=hf, X=X: e.tensor_tensor(
                                            out=X[:, hf * 512:(hf + 1) * 512], in0=banks[2 * j + hf][:], in1=X[:, hf * 512:(hf + 1) * 512], op=ALU.add),
                                            reads=['x%d' % t], writes=['x%d' % t], banks=[2 * j + hf])
                                    else:
                                        S.op('dve', lambda e, j=j, hf=hf, X=X, t=t, ex=ex: e.scalar_tensor_tensor(
                                            out=X[:, hf * 512:(hf + 1) * 512], in0=banks[2 * j + hf][:], scalar=comb[:, t, ex:ex + 1],
                                            in1=X[:, hf * 512:(hf + 1) * 512], op0=ALU.mult, op1=ALU.add),
                                            reads=['x%d' % t, 'comb'], writes=['x%d' % t], banks=[2 * j + hf])
                        if pending[0] is not None:
                            pending[0]()
                        pending[0] = down
                    for blk in blocks:
                        do_block(blk)
            if pending[0] is not None:
                pending[0]()
                pending[0] = None

        def y_out(ts):
            for t in ts:
                S.dma('sp', lambda e, t=t: e.dma_start(out=y_d[(t - 2) * 128:(t - 1) * 128, :], in_=xt(t)), reads=['x%d' % t],
                      writes=[dout_res()], chan='o')

        S.barrier()
        for t in range(2, NTILE):
            S.dma('sp', lambda e, t=t: e.dma_start(out=xt(t), in_=xin[t * 128:(t + 1) * 128, :]), writes=['x%d' % t], chan='x')

        def old_state_copies():
            for l in range(2):
                S.dma('sp', lambda e, l=l: e.dma_start(out=k_s_old_d[l], in_=sk_d[l, :, 8:128, :]), writes=[dout_res()], chan='o')
                S.dma('sp', lambda e, l=l: e.dma_start(out=v_s_old_d[l], in_=sv_d[l, :, 8:128, :]), writes=[dout_res()], chan='o')
                S.dma('sp', lambda e, l=l: e.dma_start(out=pool_s_old_d[l], in_=sp_d[l].rearrange("(b j) d -> b j d", j=15)[:, 8:15, :]),
                      writes=[dout_res()], chan='o')
        nxt_skip = set()
        for l in range(2):
            for si, seg in enumerate(SEGS):
                tiles = [t for t in seg if not (l == 1 and t == 0)]
                if not (l == 0 and si == 0):
                    load_mixer_weights(l, nxt_skip)
                if 18 in tiles:
                    for hb in range(2):
                        S.dma('sp', lambda e, hb=hb, l=l: e.dma_start(out=sptok[:, hb, :], in_=sp_d[l, hb * 120:(hb + 1) * 120, :]),
                              writes=['sptok'], chan='x')
                    for hb in range(2):
                        for c in range(2):
                            S.op('pe', lambda e, hb=hb, c=c: e.transpose(out=TRF[:, 0:120], in_=sptok[:, hb, c * 128:(c + 1) * 128],
                                                                        identity=identf[0:120, 0:120]),
                                 reads=['sptok', 'identf'], banks=[5])
                            S.op('dve', lambda e, hb=hb, c=c: e.tensor_copy(
                                out=us[:, c, hb * 8:(hb + 1) * 8, 1:16], in_=TRF[:, 0:120].rearrange("p (b j) -> p b j", j=15)),
                                writes=['us'], banks=[5])
                ffn_tiles = []
                cols = {}
                for t in tiles:
                    kv_only = (l == 0 and t == 0) or (l == 1 and t == 1)
                    cols[t] = len(ffn_tiles)
                    if not kv_only:
                        ffn_tiles.append(t)
                run_interleaved([stage1(l, tiles[0])])
                for i in range(1, len(tiles)):
                    run_interleaved([stage2(l, tiles[i - 1], cols[tiles[i - 1]]), stage1(l, tiles[i])])
                npre = 0
                run_interleaved([stage2(l, tiles[-1], cols[tiles[-1]])])
                S.barrier()
                if DEBUG:
                    for t in [tt for tt in ffn_tiles if tt in (17, 18)]:
                        S.dma('sp', lambda e, t=t, l=l: e.dma_start(out=dbg_d[2 * l, (t - 17) * 128:(t - 16) * 128, :], in_=xt(t)),
                              reads=['x%d' % t], writes=[dout_res()], chan='o')
                if l == 0 and si == 0:
                    old_state_copies()
                ffn_segment(l, ffn_tiles, npre)
                nl, nsi = (l, si + 1) if si + 1 < len(SEGS) else (l + 1, 0)
                nxt_skip = set()
                if nl < 2:
                    nxt_skip = mixer_prefetch(nl, ((1 if l == 0 else NE) * ((NFF + G - 1) // G) - 1) % NSLOT)
                if l == 1 and si == len(SEGS) - 1:
                    y_out(ffn_tiles)
                S.barrier()
                if l == 1 and si < len(SEGS) - 1:
                    y_out(ffn_tiles)
                if DEBUG:
                    for t in [tt for tt in ffn_tiles if tt in (17, 18)]:
                        S.dma('sp', lambda e, t=t, l=l: e.dma_start(out=dbg_d[2 * l + 1, (t - 17) * 128:(t - 16) * 128, :], in_=xt(t)),
                              reads=['x%d' % t], writes=[dout_res()], chan='o')
        S.barrier()
        S.emit()
        S.close()
    return nc


_NC_CACHE = {}
_DBG = {}


def _consts():
    b = np.arange(128)[:, None].astype(np.float64)
    a = np.arange(128)[None, :].astype(np.float64)
    cd = np.zeros((128, 392), np.float32)
    cv = np.zeros((128, 392), np.float32)
    cd[:, 0:128] = a - b + 128
    cv[:, 0:128] = (a < b)
    cd[:, 128:256] = np.maximum(a - b, 0)
    cv[:, 128:256] = (a >= b)
    i8 = np.arange(8)[None, :].astype(np.float64)
    cd[:, 256:264] = 128 + i8 - b
    cv[:, 256:264] = (b > i8)
    kb, kj = np.arange(128)[:, None] // 8, np.arange(128)[:, None] % 8
    qb, qi = np.arange(128)[None, :] // 8, np.arange(128)[None, :] % 8
    cd[:, 264:392] = np.maximum(qi - kj, 0)
    cv[:, 264:392] = (kb == qb) & (kj <= qi)
    return cd, cv


def _tab(first_half):
    wins = [2, 4, 8, 16]
    tab = np.zeros((128, 2, 2, 128), np.float32)
    for c in range(2):
        for hh in range(2):
            w = wins[c * 2 + hh]
            tab[hh * 64:(hh + 1) * 64, c, 1, :] = 1.0 / w
            if first_half:
                cnt = np.minimum(w, np.arange(128) + 1).astype(np.float32)
                tab[hh * 64:(hh + 1) * 64, c, 0, :] = 1.0 / cnt
            else:
                tab[hh * 64:(hh + 1) * 64, c, 0, :] = 1.0 / w
    return tab


def kernel(x_prompt, x_sample, state_pool, state_win_k, state_win_v,
           norm_mix, w_in, q_norm, k_norm, attn_sinks, pool_w, pool_scale, w_out, norm_ffn,
           ffn_w_gate, ffn_w_up, ffn_w_down, moe_router, moe_w_gate, moe_w_up, moe_w_down):
    f = lambda a: np.ascontiguousarray(np.asarray(a, dtype=np.float32))
    x_prompt, x_sample = f(x_prompt), f(x_sample)
    state_pool, state_win_k, state_win_v = f(state_pool), f(state_win_k), f(state_win_v)
    w_in = f(w_in)
    qcols = []
    for (ha, hb) in QPAIR:
        qcols += list(range(256 + ha * 64, 256 + ha * 64 + 64)) + list(range(256 + hb * 64, 256 + hb * 64 + 64))
    cols = list(range(256)) + qcols + list(range(1024, 1536))
    w_in_p = np.ascontiguousarray(w_in[:, :, cols])
    cd, cv = _consts()
    shared = {
        "cdist": cd, "cvalid": cv,
        "norm_mix": f(norm_mix), "norm_ffn": f(norm_ffn), "w_in": w_in_p, "q_norm": f(q_norm), "k_norm": f(k_norm),
        "attn_sinks": f(attn_sinks), "pool_w": f(pool_w), "pool_scale": f(pool_scale), "w_out": f(w_out),
        "ffn_w_gate": f(ffn_w_gate), "ffn_w_up": f(ffn_w_up), "ffn_w_down": f(ffn_w_down),
        "moe_router": f(moe_router), "moe_w_gate": f(moe_w_gate), "moe_w_up": f(moe_w_up), "moe_w_down": f(moe_w_down),
    }
    in_maps = []
    for c in range(NCORE):
        seq, half = c // 2, c % 2
        main = x_prompt[seq, half * 2048:(half + 1) * 2048]
        halo = x_prompt[seq, 2048 - 256:2048] if half == 1 else np.zeros((256, D), np.float32)
        xs_ = x_sample[c * 16:(c + 1) * 16].reshape(128, D)
        m = dict(shared)
        m["xin"] = np.ascontiguousarray(np.concatenate([halo, main, xs_], 0))
        m["flag"] = np.full((128, 1), float(half), np.float32)
        m["tab"] = _tab(half == 0)
        m["st_pool"] = np.ascontiguousarray(state_pool[:, c * 16:(c + 1) * 16].reshape(2, 240, 256))
        m["st_k"] = np.ascontiguousarray(state_win_k[:, c * 16:(c + 1) * 16].reshape(2, 16, 128, 256))
        m["st_v"] = np.ascontiguousarray(state_win_v[:, c * 16:(c + 1) * 16].reshape(2, 16, 128, 256))
        in_maps.append(m)
    if "nc" not in _NC_CACHE:
        _NC_CACHE["nc"] = build()
    res = run_bass_kernel_spmd(_NC_CACHE["nc"], in_maps, core_ids=list(range(NCORE)))
    R = res.results
    if DEBUG:
        _DBG["dbg"] = [np.asarray(r["dbg"]) for r in R]
        _DBG["attn"] = [np.asarray(r["dbg_attn"]).astype(np.float32) for r in R]
        _DBG["pool"] = [np.asarray(r["dbg_pool"]).astype(np.float32) for r in R]
    y_prompt = np.zeros((4, 4096, D), np.float32)
    y_sample = np.zeros((128, 8, D), np.float32)
    pool_p = np.zeros((2, 4, 15, 256), np.float32)
    k_p = np.zeros((2, 4, 128, 4, 64), np.float32)
    v_p = np.zeros((2, 4, 128, 4, 64), np.float32)
    pool_s = np.zeros((2, 128, 15, 256), np.float32)
    k_s = np.zeros((2, 128, 128, 4, 64), np.float32)
    v_s = np.zeros((2, 128, 128, 4, 64), np.float32)
    for c in range(NCORE):
        seq, half = c // 2, c % 2
        r = R[c]
        y = np.asarray(r["y"])
        y_prompt[seq, half * 2048:(half + 1) * 2048] = y[0:2048]
        y_sample[c * 16:(c + 1) * 16] = y[2048:2176].reshape(16, 8, D)
        if half == 1:
            pool_p[:, seq] = np.asarray(r["pool_p"])[:, 113:128, :]
            k_p[:, seq] = np.asarray(r["k_p"]).reshape(2, 128, 4, 64)
            v_p[:, seq] = np.asarray(r["v_p"]).reshape(2, 128, 4, 64)
        sl = slice(c * 16, (c + 1) * 16)
        pool_s[:, sl, 0:7] = np.asarray(r["pool_s_old"])
        pool_s[:, sl, 7:15] = np.asarray(r["pool_s_new"]).reshape(2, 16, 8, 256)
        k_s[:, sl, 0:120] = np.asarray(r["k_s_old"]).reshape(2, 16, 120, 4, 64)
        k_s[:, sl, 120:128] = np.asarray(r["k_s_new"]).reshape(2, 16, 8, 4, 64)
        v_s[:, sl, 0:120] = np.asarray(r["v_s_old"]).reshape(2, 16, 120, 4, 64)
        v_s[:, sl, 120:128] = np.asarray(r["v_s_new"]).reshape(2, 16, 8, 4, 64)
    return (y_prompt, y_sample, pool_p, k_p, v_p, pool_s, k_s, v_s)
```

```python
import contextlib
import math
import numpy as np
import concourse.bass as bass
import concourse.mybir as mybir
from concourse.bass_utils import run_bass_kernel_spmd

F32 = mybir.dt.float32
BF16 = mybir.dt.bfloat16
AF = mybir.ActivationFunctionType
ALU = mybir.AluOpType

D = 1024
DFF = 2816
NFF = 22
NE = 8
EPS = 1e-6
NCORE = 8
import os
DEBUG = bool(os.environ.get("KDEBUG"))
NTILE = 19
SEGS = [[0, 1, 2, 3, 4, 5, 6, 7, 8, 9], [10, 11, 12, 13, 14, 15, 16, 17, 18]]


def PAR(t):
    return t % 2


def RSLOT(t):
    return t % 3
G = 2
NSLOT = 3
QPAIR = [(0, 3), (1, 4), (2, 5), (6, 9), (7, 10), (8, 11)]
QPOS = {}
for _j, (_a, _b) in enumerate(QPAIR):
    QPOS[_a] = (_j, 0)
    QPOS[_b] = (_j, 64)


def alibi_slopes(n):
    def p2(m):
        start = 2.0 ** (-8.0 / m)
        return [start ** (i + 1) for i in range(m)]
    if float(math.log2(n)).is_integer():
        return p2(n)
    c = 2 ** int(math.floor(math.log2(n)))
    return p2(c) + p2(2 * c)[0::2][: n - c]


SLOPES = [float(np.float32(s)) for s in alibi_slopes(12)]


class Sched:
    ENG = ('pe', 'act', 'dve', 'pool', 'sp')

    def __init__(self, nc, ndma=8):
        self.nc = nc
        self.prog = {e: [] for e in self.ENG}
        self.sems = {}
        self.cnt = {e: 0 for e in self.ENG}
        self.waited = {e: {} for e in self.ENG}
        self.res = {}
        self.bank = {}
        self.ndma = ndma
        self.dma_i = {}
        self.dma_val = {}
        self.stack = []

    def sem(self, key):
        if key not in self.sems:
            cm = self.nc.semaphore("s%d" % len(self.sems))
            self.sems[key] = cm.__enter__()
            self.stack.append(cm)
        return self.sems[key]

    def _need(self, eng, tok, waits, own_ok=True):
        if tok is None:
            return
        k, v = tok
        if k == eng and (eng == 'pe' or not own_ok):
            return
        if self.waited[eng].get(k, 0) >= v:
            return
        self.waited[eng][k] = v
        waits.append((k, v))

    def _deps(self, eng, reads, writes, banks, waits):
        for r in reads:
            st = self.res.setdefault(r, [None, []])
            self._need(eng, st[0], waits)
        for w in writes:
            st = self.res.setdefault(w, [None, []])
            self._need(eng, st[0], waits)
            for t in st[1]:
                self._need(eng, t, waits, own_ok=False)
        for b in banks:
            self._need(eng, self.bank.get(b), waits, own_ok=False)

    def _commit(self, tok, reads, writes, banks):
        for r in reads:
            self.res[r][1].append(tok)
        for w in writes:
            self.res[w] = [tok, []]
        for b in banks:
            self.bank[b] = tok

    def op(self, eng, fn, reads=(), writes=(), banks=(), inc=True):
        waits = []
        self._deps(eng, reads, writes, banks, waits)
        tok = (eng, self.cnt[eng] + 1)
        if inc:
            self.cnt[eng] += 1
        self.prog[eng].append((waits, fn, (eng, 1) if inc else None))
        self._commit(tok, reads, writes, banks)
        return tok

    def dma(self, q, fn, reads=(), writes=(), chan='d'):
        waits = []
        self._deps(q, reads, writes, (), waits)
        i = self.dma_i.get(chan, 0)
        self.dma_i[chan] = i + 1
        key = ('dma', chan, i % self.ndma)
        prev = self.dma_val.get(key, 0)
        if prev:
            self._need(q, (key, prev), waits)
        val = prev + 16
        self.dma_val[key] = val
        tok = (key, val)
        self.prog[q].append((waits, fn, (key, 16)))
        self._commit(tok, reads, writes, ())
        return tok

    def barrier(self):
        toks = [(e, self.cnt[e]) for e in self.ENG if self.cnt[e] > 0]
        toks += [(k, v) for k, v in self.dma_val.items()]
        for e in self.ENG:
            waits = []
            for t in toks:
                self._need(e, t, waits, own_ok=False)
            if waits:
                self.prog[e].append((waits, None, None))

    def emit(self):
        nc = self.nc
        for e in self.ENG:
            self.sem(e)
            for waits, fn, inc in self.prog[e]:
                for k, v in waits:
                    self.sem(k)
                if inc is not None:
                    self.sem(inc[0])
        engobj = {'pe': 'tensor', 'act': 'scalar', 'dve': 'vector', 'pool': 'gpsimd', 'sp': 'sync'}
        with nc.allow_non_contiguous_dma(reason="tiny strided parameter loads"), nc.Block() as block:
            for e in self.ENG:
                def body(engine, e=e):
                    for waits, fn, inc in self.prog[e]:
                        for k, v in waits:
                            engine.wait_ge(self.sems[k], v)
                        if fn is not None:
                            ins = fn(engine)
                            if inc is not None:
                                ins.then_inc(self.sems[inc[0]], inc[1])
                getattr(block, engobj[e])(body)

    def close(self):
        for cm in reversed(self.stack):
            cm.__exit__(None, None, None)


def build():
    nc = bass.Bass("TRN2", target_bir_lowering=False)

    def din(name, shape):
        return nc.dram_tensor(name, list(shape), F32, kind="ExternalInput").ap()

    def dout(name, shape):
        return nc.dram_tensor(name, list(shape), F32, kind="ExternalOutput").ap()

    xin = din("xin", [NTILE * 128, D])
    flag_d = din("flag", [128, 1])
    cdist_d = din("cdist", [128, 392])
    cvalid_d = din("cvalid", [128, 392])
    tab_d = din("tab", [128, 2, 2, 128])
    sp_d = din("st_pool", [2, 240, 256])
    sk_d = din("st_k", [2, 16, 128, 256])
    sv_d = din("st_v", [2, 16, 128, 256])
    norm_mix_d = din("norm_mix", [2, D])
    norm_ffn_d = din("norm_ffn", [2, D])
    w_in_d = din("w_in", [2, D, 1536])
    qn_d = din("q_norm", [2, 64])
    kn_d = din("k_norm", [2, 64])
    sinks_d = din("attn_sinks", [2, 12])
    pool_w_d = din("pool_w", [2, 4, 64, 64])
    pool_scale_d = din("pool_scale", [2, 256])
    w_out_d = din("w_out", [2, D, D])
    fg_d = din("ffn_w_gate", [1, D, DFF])
    fu_d = din("ffn_w_up", [1, D, DFF])
    fd_d = din("ffn_w_down", [1, DFF, D])
    router_d = din("moe_router", [1, D, NE])
    mg_d = din("moe_w_gate", [1, NE, D, DFF])
    mu_d = din("moe_w_up", [1, NE, D, DFF])
    md_d = din("moe_w_down", [1, NE, DFF, D])

    y_d = dout("y", [17 * 128, D])
    pool_p_d = dout("pool_p", [2, 128, 256])
    k_p_d = dout("k_p", [2, 128, 256])
    v_p_d = dout("v_p", [2, 128, 256])
    pool_s_new_d = dout("pool_s_new", [2, 128, 256])
    pool_s_old_d = dout("pool_s_old", [2, 16, 7, 256])
    k_s_new_d = dout("k_s_new", [2, 128, 256])
    k_s_old_d = dout("k_s_old", [2, 16, 120, 256])
    v_s_new_d = dout("v_s_new", [2, 128, 256])
    v_s_old_d = dout("v_s_old", [2, 16, 120, 256])

    dbg_d = dout("dbg", [4, 2 * 128, D]) if DEBUG else None
    dbg_attn = nc.dram_tensor("dbg_attn", [128, 768], BF16, kind="ExternalOutput").ap() if DEBUG else None
    dbg_pool = nc.dram_tensor("dbg_pool", [128, 2, 128], BF16, kind="ExternalOutput").ap() if DEBUG else None
    S = Sched(nc)
    _dout_n = [0]

    def dout_res():
        _dout_n[0] += 1
        return 'dram_out%d' % _dout_n[0]
    with contextlib.ExitStack() as es:
        def sb(name, shape, dt):
            return es.enter_context(nc.sbuf_tensor(name, list(shape), dt))

        x_sb = sb("x_sb", [128, 18, D], F32)
        hT = sb("hT", [128, 8, 9 * 128], BF16)
        x0_v = hT[:, 6:8, :].rearrange("p c n -> p (c n)").bitcast(F32)[:, 0:D]
        wbuf = sb("wbuf", [128, 20480], BF16)
        Mall = sb("Mall", [128, 12, 392], BF16)
        ident = sb("ident", [128, 128], BF16)
        identf = sb("identf", [128, 128], F32)
        bones = sb("bones", [128, 128], BF16)
        hmT = sb("hmT", [128, 8, 128], BF16)
        scr = sb("scr", [128, 2304], BF16)
        xs = scr[:, 0:1024]
        xs2 = scr[:, 1024:2048]
        sq = sb("sq", [128, 8, 128], BF16)
        rs = sb("rs", [128, 8, 128], F32)
        qnb = [sb("qn%d" % i, [128, 6, 128], BF16) for i in range(2)]
        kT = [sb("kT%d" % i, [128, 2, 128], BF16) for i in range(3)]
        kf = sb("kf", [128, 2, 128], F32)
        Vaug = [sb("Vaug%d" % i, [128, 4, 65], BF16) for i in range(3)]
        vf = sb("vf", [128, 256], F32)
        uextb = [sb("uext%d" % i, [128, 2, 144], F32) for i in range(2)]
        sA = sb("sA", [128, 768], F32)
        sBb = sb("sBb", [128, 768], F32)
        cdist = sA[:, 0:392]
        cvalid = sBb[:, 0:392]
        tmpM = rs[:].rearrange("p c n -> p (c n)")[:, 0:392]
        Wk = sb("Wk", [128, 2, 128], F32)
        bonesf = Wk[:, 0, :]
        pooledb = [sb("pooled%d" % i, [128, 2, 128], BF16) for i in range(2)]
        PTg = [sb("PTg%d" % i, [128, 768], BF16) for i in range(2)]
        PT = [PTg[i][:, 0:256].rearrange("p (c n) -> p c n", c=2) for i in range(2)]
        PTn = PTg[0][:, 256:384]
        mixtok = sb("mixtok", [128, 768], BF16)
        mixT = sb("mixT", [128, 8, 128], BF16)
        tab = sb("tab_s", [128, 2, 2, 128], F32)
        pwbd = sb("pwbd", [128, 2, 128], BF16)
        flag = sb("flag_s", [128, 1], F32)
        fence = sb("fence", [128, 1], F32)
        gmixT = sb("gmixT", [128, 2, 8], F32)
        gffnT = sb("gffnT", [128, 2, 8], F32)
        gq = sb("gq", [128, 2], F32)
        gk = sb("gk", [128, 2], F32)
        pscale = sb("pscale", [128, 2, 2], F32)
        esink = sb("esink", [128, 2, 12], F32)
        ss1 = sb("ss1", [128, 1], F32)
        rstd1 = sb("rstd1", [128, 1], F32)
        ss2 = sb("ss2", [128, 1], F32)
        rstd2 = sb("rstd2", [128, 1], F32)
        den = sb("den", [128, 12], F32)
        rden = sb("rden", [128, 12], F32)
        routerw = sb("routerw", [128, 8, NE], BF16)
        lg = sb("lg", [128, NE], F32)
        lg2 = sb("lg2", [128, NE], F32)
        msk = sb("msk", [128, NE], F32)
        m1 = sb("m1", [128, 1], F32)
        m2 = sb("m2", [128, 1], F32)
        nm1 = sb("nm1", [128, 1], F32)
        rr = sb("rr", [128, 1], F32)
        comb = sb("comb", [128, 19, NE], F32)
        ost = sb("ost", [128, 512], F32)
        outst = [ost[:, i * 256:(i + 1) * 256] for i in range(2)]
        sptok = ost[0:120, :].rearrange("p (h n) -> p h n", h=2)
        ktok = [sb("ktok%d" % i, [128, 256], BF16) for i in range(2)]
        kTs = sb("kTs", [128, 2, 16, 128], BF16)
        Vs = sb("Vs", [128, 16, 4, 65], BF16)
        PTz = sb("PTz", [128, 16, 128], BF16)
        us = sb("us", [128, 2, 16, 24], F32)
        sg = [scr[:, i * 384:(i + 1) * 384] for i in range(2)]
        aT = [scr[:, 768 + i * 768:768 + (i + 1) * 768].rearrange("p (g n) -> p g n", g=G) for i in range(2)]

        banks = [es.enter_context(nc.psum_tensor("bank%d" % i, [128, 512], F32)) for i in range(8)]

        Win = wbuf[:, 0:12288].rearrange("p (k n) -> p k n", k=8)
        Wout = wbuf[:, 12288:20480].rearrange("p (k n) -> p k n", k=8)
        SLOTSZ = 6144
        def slot_views(s):
            base = s * SLOTSZ
            wg = wbuf[:, base:base + 2048].rearrange("p (k n) -> p k n", k=8)
            wu = wbuf[:, base + 2048:base + 4096].rearrange("p (k n) -> p k n", k=8)
            wd = wbuf[:, base + 4096:base + 6144].rearrange("p (g n) -> p g n", g=G)
            return wg, wu, wd

        def xt(t):
            return x0_v if t == 0 else x_sb[:, t - 1, :]

        QKh = banks[0][:].rearrange("p (c n) -> p c n", c=4)
        SSh = banks[1][:].rearrange("p (c n) -> p c n", c=4)
        U_ps = banks[4][:, 0:256].rearrange("p (c n) -> p c n", c=2)
        V_ps = banks[4][:, 256:512]
        TR = banks[5][:].bitcast(BF16).rearrange("p (c n) -> p c n", c=8)
        TRF = banks[5][:]
        TR2 = banks[6][:].bitcast(BF16).rearrange("p (c n) -> p c n", c=8)
        TRF2 = banks[6][:]
        ST = [banks[6][:, 0:256].rearrange("p (c n) -> p c n", c=2), banks[7][:, 0:256].rearrange("p (c n) -> p c n", c=2)]
        def o_ps(h):
            return banks[2 + h // 6][:, (h % 6) * 65:(h % 6) * 65 + 65]
        sAv = sA[:, 0:288].rearrange("p (c n) -> p c n", c=2)
        sBv = sBb[:, 0:288].rearrange("p (c n) -> p c n", c=2)
        sAs = sA[:].rearrange("p (c b n) -> p c b n", c=2, b=16)
        sBs = sBb[:].rearrange("p (c b n) -> p c b n", c=2, b=16)

        def load_mixer_weights(l, skip=()):
            for k in range(8):
                if ('in', k) in skip:
                    continue
                S.dma('pool', lambda e, k=k: e.dma_start(out=Win[:, k, :], in_=w_in_d[l, k * 128:(k + 1) * 128, :]),
                      writes=['Win%d' % k], chan='w')
            for k in range(8):
                if ('out', k) in skip:
                    continue
                S.dma('pool', lambda e, k=k: e.dma_start(out=Wout[:, k, :], in_=w_out_d[l, k * 128:(k + 1) * 128, :]),
                      writes=['Wout%d' % k], chan='w')
            S.op('pool', lambda e: e.memset(pwbd[:], 0.0), writes=['pwbd'])
            for g in range(4):
                c, po = g // 2, (g % 2) * 64
                S.dma('pool', lambda e, g=g, c=c, po=po: e.dma_start(out=pwbd[po:po + 64, c, po:po + 64], in_=pool_w_d[l, g]),
                      reads=[], writes=['pwbd'], chan='w')

        load_mixer_weights(0)
        S.dma('sp', lambda e: e.dma_start(out=cdist, in_=cdist_d), writes=['cdist'])
        S.dma('sp', lambda e: e.dma_start(out=cvalid, in_=cvalid_d), writes=['cvalid'])
        S.dma('sp', lambda e: e.dma_start(out=tab[:], in_=tab_d), writes=['tab'])
        S.dma('sp', lambda e: e.dma_start(out=flag[:], in_=flag_d), writes=['flag'])
        for l in range(2):
            S.dma('sp', lambda e, l=l: e.dma_start(out=gmixT[:, l, :], in_=norm_mix_d[l].rearrange("(c p) -> p c", p=128)),
                  writes=['gmixT'])
            S.dma('sp', lambda e, l=l: e.dma_start(out=gffnT[:, l, :], in_=norm_ffn_d[l].rearrange("(c p) -> p c", p=128)),
                  writes=['gffnT'])
            for hh in range(2):
                S.dma('sp', lambda e, l=l, hh=hh: e.dma_start(out=gq[hh * 64:(hh + 1) * 64, l:l + 1],
                                                            in_=qn_d[l].rearrange("(p o) -> p o", o=1)), writes=['gq'])
                S.dma('sp', lambda e, l=l, hh=hh: e.dma_start(out=gk[hh * 64:(hh + 1) * 64, l:l + 1],
                                                            in_=kn_d[l].rearrange("(p o) -> p o", o=1)), writes=['gk'])
            S.dma('sp', lambda e, l=l: e.dma_start(out=pscale[:, l, :], in_=pool_scale_d[l].rearrange("(c p) -> p c", p=128)),
                  writes=['pscale'])
            S.dma('sp', lambda e, l=l: e.dma_start(out=esink[:, l, :], in_=sinks_d[l:l + 1, :].partition_broadcast(128)),
                  writes=['esink'])
        S.op('act', lambda e: e.activation(out=esink[:], in_=esink[:], func=AF.Exp), reads=['esink'], writes=['esink'])
        S.op('dve', lambda e: e.tensor_scalar(out=gq[:], in0=gq[:], scalar1=0.125, scalar2=None, op0=ALU.mult),
             reads=['gq'], writes=['gq'])
        S.op('pool', lambda e: e.memset(identf[:], 1.0), writes=['identf'])
        S.op('pool', lambda e: e.affine_select(out=identf[:], in_=identf[:], pattern=[[-1, 128]],
                                              compare_op=ALU.is_equal, fill=0.0, base=0, channel_multiplier=1),
             reads=['identf'], writes=['identf'])
        S.op('pool', lambda e: e.tensor_copy(out=ident[:], in_=identf[:]), reads=['identf'], writes=['ident'])
        S.op('pool', lambda e: e.memset(bonesf, 0.0), writes=['bonesf'])
        S.op('pool', lambda e: e.memset(bonesf[0:64, 0:64], 1.0 / 64), reads=['bonesf'], writes=['bonesf'])
        S.op('pool', lambda e: e.memset(bonesf[64:128, 64:128], 1.0 / 64), reads=['bonesf'], writes=['bonesf'])
        S.op('pool', lambda e: e.tensor_copy(out=bones[:], in_=bonesf), reads=['bonesf'], writes=['bones'])
        S.op('pool', lambda e: e.memset(PTz[:], 0.0), writes=['PTz'])
        for i in range(3):
            S.op('pool', lambda e, i=i: e.memset(Vaug[i][:], 1.0), writes=['Vaug%d' % i])
        S.op('pool', lambda e: e.memset(Vs[:], 1.0), writes=['Vs'])
        S.op('pool', lambda e: e.memset(us[:], 0.0), writes=['us'])
        for i in range(2):
            S.op('pool', lambda e, i=i: e.memset(uextb[i][:], 0.0), writes=['uext%d' % i])
        for h in range(12):
            S.op('act', lambda e, h=h: e.activation(out=tmpM, in_=cdist, func=AF.Exp, scale=-SLOPES[h]),
                 reads=['cdist'], writes=['tmpM'])
            S.op('dve', lambda e, h=h: e.tensor_tensor(out=Mall[:, h, :], in0=tmpM, in1=cvalid, op=ALU.mult),
                 reads=['tmpM', 'cvalid'], writes=['Mall'])
        for t in range(1):
            S.dma('sp', lambda e, t=t: e.dma_start(out=xt(t), in_=xin[t * 128:(t + 1) * 128, :]), writes=['x%d' % t], chan='x')

        def norm_T(t, gT_l, dst, dst_res, which):
            X = xt(t)
            xr = 'x%d' % t
            xsb, ssb, rsb = ((xs, ss1, rstd1), (xs2, ss2, rstd2))[which]
            xn, sn, rn = 'xs%d' % which, 'ss%d' % which, 'rstd%d' % which
            TRw, bk = ((TR, 5), (TR2, 6))[which]
            S.op('act', lambda e: e.activation(out=xsb, in_=X, func=AF.Square, accum_out=ssb[:]),
                 reads=[xr], writes=[xn, sn])
            S.op('act', lambda e: e.activation(out=rsb[:], in_=ssb[:], func=AF.Ln, scale=1.0 / D, bias=EPS),
                 reads=[sn], writes=[rn])
            S.op('act', lambda e: e.activation(out=rsb[:], in_=rsb[:], func=AF.Exp, scale=-0.5),
                 reads=[rn], writes=[rn])
            yield
            S.op('dve', lambda e: e.tensor_scalar(out=xsb, in0=X, scalar1=rsb[:, 0:1], scalar2=None, op0=ALU.mult),
                 reads=[xr, rn], writes=[xn])
            yield
            for c in range(8):
                S.op('pe', lambda e, c=c: e.transpose(out=TRw[:, c, :], in_=xsb[:, c * 128:(c + 1) * 128], identity=ident[:]),
                     reads=[xn, 'ident'], banks=[bk], inc=(c == 7))
            yield
            S.op('dve', lambda e: e.tensor_tensor(out=dst, in0=TRw, in1=gT_l.unsqueeze(2).broadcast_to([128, 8, 128]),
                                                 op=ALU.mult), reads=['gmixT', 'gffnT'], writes=[dst_res], banks=[bk])
            yield

        def project(l, t, r, halo, want_state):
            qn = qnb[PAR(t)]
            qres = 'qn%d' % PAR(t)
            yield from norm_T(t, gmixT[:, l, :], hmT[:], 'hmT', 0)
            for half in range(2):
                for cc in range(4):
                    c = half * 4 + cc
                    for k in range(8):
                        S.op('pe', lambda e, c=c, cc=cc, k=k: e.matmul(QKh[:, cc, :], lhsT=Win[:, k, 256 + c * 128:256 + (c + 1) * 128],
                                                                      rhs=hmT[:, k, :], start=(k == 0), stop=(k == 7)),
                             reads=['hmT', 'Win%d' % k], banks=[0], inc=(k == 7 and cc == 3))
                yield
                if half == 0:
                    for c in range(2):
                        for k in range(8):
                            S.op('pe', lambda e, c=c, k=k: e.matmul(U_ps[:, c, :], lhsT=Win[:, k, c * 128:(c + 1) * 128],
                                                                   rhs=hmT[:, k, :], start=(k == 0), stop=(k == 7)),
                                 reads=['hmT', 'Win%d' % k], banks=[4], inc=False)
                    for k in range(8):
                        S.op('pe', lambda e, k=k: e.matmul(V_ps, lhsT=hmT[:, k, :], rhs=Win[:, k, 1280:1536],
                                                          start=(k == 0), stop=(k == 7)),
                             reads=['hmT', 'Win%d' % k], banks=[4], inc=(k == 7))
                S.op('act', lambda e, half=half: e.activation(out=sq[:, 4 * half:4 * half + 4, :], in_=QKh, func=AF.Square),
                     writes=['sq%d' % half], banks=[0])
                yield
                for cc in range(4):
                    S.op('pe', lambda e, cc=cc, half=half: e.matmul(SSh[:, cc, :], lhsT=bones[:], rhs=sq[:, 4 * half + cc, :],
                                                                   start=True, stop=True),
                         reads=['sq%d' % half, 'bones'], banks=[1], inc=(cc == 3))
                yield
                S.op('act', lambda e, half=half: e.activation(out=rs[:, 4 * half:4 * half + 4, :], in_=SSh, func=AF.Ln, bias=EPS),
                     writes=['rs%d' % half], banks=[1])
                S.op('act', lambda e, half=half: e.activation(out=rs[:, 4 * half:4 * half + 4, :], in_=rs[:, 4 * half:4 * half + 4, :],
                                                             func=AF.Exp, scale=-0.5), reads=['rs%d' % half], writes=['rs%d' % half])
                yield
                if half == 0:
                    S.op('dve', lambda e: e.scalar_tensor_tensor(out=qn[:, 0:4, :], in0=QKh, scalar=gq[:, l:l + 1], in1=rs[:, 0:4, :],
                                                                op0=ALU.mult, op1=ALU.mult),
                         reads=['rs0', 'gq'], writes=[qres], banks=[0])
                else:
                    S.op('dve', lambda e: e.scalar_tensor_tensor(out=qn[:, 4:6, :], in0=QKh[:, 0:2, :], scalar=gq[:, l:l + 1],
                                                                in1=rs[:, 4:6, :], op0=ALU.mult, op1=ALU.mult),
                         reads=['rs1', 'gq'], writes=[qres], banks=[0])
                    S.op('dve', lambda e: e.scalar_tensor_tensor(out=kT[r][:], in0=QKh[:, 2:4, :], scalar=gk[:, l:l + 1],
                                                                in1=rs[:, 6:8, :], op0=ALU.mult, op1=ALU.mult),
                         reads=['rs1', 'gk'], writes=['kT%d' % r], banks=[0])
                    if want_state:
                        S.op('dve', lambda e: e.scalar_tensor_tensor(out=kf[:], in0=QKh[:, 2:4, :], scalar=gk[:, l:l + 1],
                                                                    in1=rs[:, 6:8, :], op0=ALU.mult, op1=ALU.mult),
                             reads=['rs1', 'gk'], writes=['kf'], banks=[0])
                yield
            vsrc = V_ps.rearrange("p (g d) -> p g d", g=4)
            if halo:
                S.op('act', lambda e: e.activation(out=Vaug[r][:, :, 64:65], in_=Vaug[r][:, :, 64:65], func=AF.Identity, scale=0.0,
                                                  bias=flag[:, 0:1]), reads=['flag'], writes=['Vaug%d' % r])
                S.op('act', lambda e: e.activation(out=Vaug[r][:, :, 0:64], in_=vsrc, func=AF.Copy, scale=flag[:, 0:1]),
                     reads=['flag'], writes=['Vaug%d' % r], banks=[4])
            else:
                S.op('act', lambda e: e.activation(out=Vaug[r][:, :, 64:65], in_=Vaug[r][:, :, 64:65], func=AF.Copy, scale=0.0,
                                                  bias=1.0), writes=['Vaug%d' % r])
                S.op('act', lambda e: e.activation(out=Vaug[r][:, :, 0:64], in_=vsrc, func=AF.Copy),
                     writes=['Vaug%d' % r], banks=[4])
            if want_state:
                S.op('dve', lambda e: e.tensor_copy(out=vf[:], in_=V_ps), writes=['vf'], banks=[4])
            if t != 18:
                S.op('act', lambda e: e.activation(out=uextb[t % 2][:, :, 16:144], in_=U_ps, func=AF.Copy),
                     writes=['uext%d' % (t % 2)], banks=[4])
            else:
                for c in range(2):
                    S.op('act', lambda e, c=c: e.activation(out=us[:, c, :, 16:24], in_=U_ps[:, c, :].rearrange("p (b i) -> p b i", b=16),
                                                           func=AF.Copy), writes=['us'], banks=[4])
            yield

        def pool_sums(src, A, B, n, tabv, sample, srcres='us'):
            def sl(v, a, b):
                return v[:, :, :, a:b] if sample else v[:, :, a:b]
            def pick(v, p0, c):
                o = 16
                return (v[p0:p0 + 64, c, :, o:o + 8] if sample else v[p0:p0 + 64, c, o:o + 128])
            def wk(p0, c):
                return (Wk[p0:p0 + 64, c, :].rearrange("p (b i) -> p b i", b=16) if sample else Wk[p0:p0 + 64, c, :])
            def tb(p0, c):
                return (tabv[p0:p0 + 64, c, :].rearrange("p (b i) -> p b i", b=16) if sample else tabv[p0:p0 + 64, c, :])
            srcr = 'us' if sample else srcres
            def shift_add(dst, srcv, lo, sh, rres, wres):
                if sample:
                    for c in range(2):
                        S.op('pool', lambda e, c=c: e.tensor_tensor(out=dst[:, c, :, lo:n], in0=srcv[:, c, :, lo:n],
                                                                   in1=srcv[:, c, :, lo - sh:n - sh], op=ALU.add),
                             reads=[rres], writes=[wres])
                else:
                    S.op('pool', lambda e: e.tensor_tensor(out=dst[:, :, lo:n], in0=srcv[:, :, lo:n], in1=srcv[:, :, lo - sh:n - sh],
                                                          op=ALU.add), reads=[rres], writes=[wres])
            shift_add(A, src, 1, 1, srcr, 'sA')
            S.op('pool', lambda e: e.tensor_tensor(out=wk(0, 0), in0=pick(A, 0, 0), in1=tb(0, 0), op=ALU.mult),
                 reads=['sA', 'tab'], writes=['Wk'])
            yield
            shift_add(B, A, 3, 2, 'sA', 'sB')
            S.op('pool', lambda e: e.tensor_tensor(out=wk(64, 0), in0=pick(B, 64, 0), in1=tb(64, 0), op=ALU.mult),
                 reads=['sB', 'tab'], writes=['Wk'])
            yield
            shift_add(A, B, 7, 4, 'sB', 'sA')
            S.op('pool', lambda e: e.tensor_tensor(out=wk(0, 1), in0=pick(A, 0, 1), in1=tb(0, 1), op=ALU.mult),
                 reads=['sA', 'tab'], writes=['Wk'])
            yield
            shift_add(B, A, 15, 8, 'sA', 'sB')
            S.op('pool', lambda e: e.tensor_tensor(out=wk(64, 1), in0=pick(B, 64, 1), in1=tb(64, 1), op=ALU.mult),
                 reads=['sB', 'tab'], writes=['Wk'])

        def pool_mix(l, t):
            sample = (t == 18)
            pooled = pooledb[PAR(t)]
            pres = 'pooled%d' % PAR(t)
            tabv = tab[:, :, 0, :] if t == 2 else tab[:, :, 1, :]
            if sample:
                yield from pool_sums(us[:], sAs, sBs, 24, tabv, True)
                for c in range(2):
                    S.op('pool', lambda e, c=c: e.tensor_tensor(out=pooled[:, c, :].rearrange("p (b i) -> p b i", b=16),
                                                               in0=Wk[:, c, :].rearrange("p (b i) -> p b i", b=16),
                                                               in1=us[:, c, :, 16:24], op=ALU.subtract),
                         reads=['Wk', 'us'], writes=[pres])
            else:
                ue, uep = uextb[t % 2], uextb[(t - 1) % 2]
                un, unp = 'uext%d' % (t % 2), 'uext%d' % ((t - 1) % 2)
                S.op('pool', lambda e: e.tensor_copy(out=ue[:, :, 0:16], in_=uep[:, :, 128:144]), reads=[unp, un], writes=[un])
                yield from pool_sums(ue[:], sAv, sBv, 144, tabv, False, un)
                S.op('pool', lambda e: e.tensor_tensor(out=pooled[:], in0=Wk[:], in1=ue[:, :, 16:144], op=ALU.subtract),
                     reads=['Wk', un], writes=[pres])
            yield

        def pool_project(l, t):
            pooled = pooledb[PAR(t)]
            pres = 'pooled%d' % PAR(t)
            PM = TRF2[:, 0:256].rearrange("p (c n) -> p c n", c=2)
            for c in range(2):
                S.op('pe', lambda e, c=c: e.matmul(PM[:, c, :], lhsT=pwbd[:, c, :], rhs=pooled[:, c, :], start=True, stop=True),
                     reads=[pres, 'pwbd'], banks=[6], inc=(c == 1))
            for c in range(2):
                S.op('act', lambda e, c=c: e.activation(out=mixT[:, c, :], in_=PM[:, c, :], func=AF.Copy, scale=pscale[:, l, c:c + 1]),
                     reads=['pscale'], writes=['mixT'], banks=[6])
            yield

        def fp32_T_out(src_fn, dst_dram_fn, rows, src_res, oi):
            for c in range(2):
                S.op('pe', lambda e, c=c: e.transpose(out=TRF[:, 256 + c * 128:256 + (c + 1) * 128], in_=src_fn(c), identity=identf[:]),
                     reads=[src_res, 'identf'], banks=[5], inc=(c == 1))
            S.op('dve', lambda e: e.tensor_copy(out=outst[oi], in_=TRF[:, 256:512]), writes=['outst%d' % oi], banks=[5])
            S.dma('sp', lambda e: e.dma_start(out=dst_dram_fn(), in_=outst[oi][rows[0]:rows[1], :]), reads=['outst%d' % oi],
                  writes=[dout_res()], chan='o')

        def attention_prompt(l, t, r):
            rp = (t - 1) % 3
            qn = qnb[t % 2]
            qres = 'qn%d' % (t % 2)
            STp = banks[6][:, 0:384].rearrange("p (h n) -> p h n", h=3)
            STc = banks[7][:, 0:384].rearrange("p (h n) -> p h n", h=3)
            def s_mm(g):
                po = (g % 2) * 64
                kc = g // 2
                qc0 = QPOS[3 * g][0]
                pb = g % 2
                P4 = PTg[pb][:].rearrange("p (c h n) -> p c h n", c=2, h=3)
                pres = 'PTg%d' % pb
                S.op('pe', lambda e: e.matmul(STp, lhsT=kT[rp][po:po + 64, kc, :], rhs=qn[po:po + 64, qc0:qc0 + 3, :],
                                              start=True, stop=True), reads=['kT%d' % rp, qres], banks=[6], inc=True)
                S.op('pe', lambda e: e.matmul(STc, lhsT=kT[r][po:po + 64, kc, :], rhs=qn[po:po + 64, qc0:qc0 + 3, :],
                                              start=True, stop=True), reads=['kT%d' % r, qres], banks=[7], inc=True)
                S.op('act', lambda e: e.activation(out=P4[:, 0], in_=STp, func=AF.Exp), writes=[pres + 'p'], banks=[6])
                S.op('act', lambda e: e.activation(out=P4[:, 1], in_=STc, func=AF.Exp), writes=[pres + 'c'], banks=[7])
                S.op('dve', lambda e: e.tensor_tensor(out=P4[:, 0], in0=P4[:, 0], in1=Mall[:, 3 * g:3 * g + 3, 0:128], op=ALU.mult),
                     reads=[pres + 'p', 'Mall'], writes=[pres + 'p'])
                S.op('dve', lambda e: e.tensor_tensor(out=P4[:, 1], in0=P4[:, 1], in1=Mall[:, 3 * g:3 * g + 3, 128:256], op=ALU.mult),
                     reads=[pres + 'c', 'Mall'], writes=[pres + 'c'])
            def pv_mm(g):
                pb = g % 2
                P4 = PTg[pb][:].rearrange("p (c h n) -> p c h n", c=2, h=3)
                pres = 'PTg%d' % pb
                for hh in range(3):
                    h = 3 * g + hh
                    S.op('pe', lambda e, h=h, hh=hh: e.matmul(o_ps(h), lhsT=P4[:, 0, hh, :], rhs=Vaug[rp][:, g, :], start=True, stop=False),
                         reads=[pres + 'p', 'Vaug%d' % rp], banks=[2 + h // 6], inc=False)
                    S.op('pe', lambda e, h=h, hh=hh: e.matmul(o_ps(h), lhsT=P4[:, 1, hh, :], rhs=Vaug[r][:, g, :], start=False, stop=True),
                         reads=[pres + 'c', 'Vaug%d' % r], banks=[2 + h // 6], inc=(hh == 2))
            s_mm(0)
            yield
            for g in range(4):
                if g + 1 < 4:
                    s_mm(g + 1)
                    yield
                pv_mm(g)
                yield

        def sample_kv_load(l, pair):
            for b in (2 * pair, 2 * pair + 1):
                S.dma('pool', lambda e, b=b: e.dma_start(out=Vs[:, b, :, 0:64], in_=sv_d[l, b].rearrange("k (g d) -> k g d", g=4)),
                      writes=['Vs'], chan='w')
                kb = ktok[b % 2]
                S.dma('pool', lambda e, b=b, kb=kb: e.dma_start(out=kb[:], in_=sk_d[l, b]), writes=['ktok%d' % (b % 2)], chan='w')

        def sample_k_transpose(l, pair):
            for b in (2 * pair, 2 * pair + 1):
                kb = ktok[b % 2]
                for c in range(2):
                    S.op('pe', lambda e, c=c, kb=kb: e.transpose(out=TR2[:, c, :], in_=kb[:, c * 128:(c + 1) * 128], identity=ident[:]),
                         reads=['ktok%d' % (b % 2), 'ident'], banks=[6], inc=(c == 1))
                S.op('act', lambda e, b=b: e.activation(out=kTs[:, :, b, :], in_=TR2[:, 0:2, :], func=AF.Copy),
                     writes=['kTs'], banks=[6])

        def attention_sample(l):
            r = RSLOT(18)
            qn = qnb[PAR(18)]
            qres = 'qn%d' % PAR(18)
            def head(h):
                g = h // 3
                qc, po = QPOS[h]
                kc = g // 2
                sbi = h % 2
                stb = ST[sbi]
                sflat = banks[6 + sbi][:, 0:256]
                for b in range(16):
                    S.op('pe', lambda e, b=b: e.matmul(sflat[:, b * 8:(b + 1) * 8], lhsT=kTs[po:po + 64, kc, b, :],
                                                      rhs=qn[po:po + 64, qc, b * 8:(b + 1) * 8], start=True, stop=True),
                         reads=['kTs', qres], banks=[6 + sbi], inc=False)
                S.op('pe', lambda e: e.matmul(sflat[:, 128:256], lhsT=kT[r][po:po + 64, kc, :], rhs=qn[po:po + 64, qc, :],
                                              start=True, stop=True), reads=['kT%d' % r, qres], banks=[6 + sbi], inc=True)
                S.op('act', lambda e: e.activation(out=PT[sbi], in_=stb, func=AF.Exp), writes=['PT%d' % sbi], banks=[6 + sbi])
                diag = bass.AP(PTz[:].tensor, PTz[:].offset, [list(PTz[:].ap[0]), [128 + 8, 16], [1, 8]])
                S.op('dve', lambda e, diag=diag: e.tensor_tensor(
                    out=diag, in0=PT[sbi][:, 0, :].rearrange("p (b i) -> p b i", b=16),
                    in1=Mall[:, h, 256:264].unsqueeze(1).broadcast_to([128, 16, 8]), op=ALU.mult),
                    reads=['PT%d' % sbi, 'Mall'], writes=['PTz'])
                S.op('dve', lambda e: e.tensor_tensor(out=PTn, in0=PT[sbi][:, 1, :], in1=Mall[:, h, 264:392], op=ALU.mult),
                     reads=['PT%d' % sbi, 'Mall'], writes=['PTn'])
                for b in range(16):
                    S.op('pe', lambda e, b=b: e.matmul(o_ps(h), lhsT=PTz[:, b, :], rhs=Vs[:, b, g, :], start=(b == 0), stop=False),
                         reads=['PTz', 'Vs'], banks=[2 + h // 6], inc=False)
                S.op('pe', lambda e: e.matmul(o_ps(h), lhsT=PTn, rhs=Vaug[r][:, g, :], start=False, stop=True),
                     reads=['PTn', 'Vaug%d' % r], banks=[2 + h // 6], inc=True)
            for h in range(12):
                head(h)
                yield

        def attn_finish(l, t):
            for b in range(2):
                ob = banks[2 + b][:, 0:390].rearrange("p (h d) -> p h d", h=6)
                S.op('dve', lambda e, b=b, ob=ob: e.tensor_tensor(out=den[:, 6 * b:6 * b + 6], in0=ob[:, :, 64],
                                                                 in1=esink[:, l, 6 * b:6 * b + 6], op=ALU.add),
                     reads=['esink'], writes=['den%d' % b], banks=[2 + b])
                S.op('dve', lambda e, b=b: e.reciprocal(out=rden[:, 6 * b:6 * b + 6], in_=den[:, 6 * b:6 * b + 6]),
                     reads=['den%d' % b], writes=['rden%d' % b])
                S.op('dve', lambda e, b=b, ob=ob: e.tensor_tensor(
                    out=mixtok[:, 384 * b:384 * b + 384].rearrange("p (h d) -> p h d", h=6), in0=ob[:, :, 0:64],
                    in1=rden[:, 6 * b:6 * b + 6].unsqueeze(2).broadcast_to([128, 6, 64]), op=ALU.mult),
                    reads=['rden%d' % b], writes=['mixtok'], banks=[2 + b])
            yield
            for c in range(6):
                S.op('pe', lambda e, c=c: e.transpose(out=TR2[:, c, :], in_=mixtok[:, c * 128:(c + 1) * 128], identity=ident[:]),
                     reads=['mixtok', 'ident'], banks=[6], inc=(c == 5))
            S.op('act', lambda e: e.activation(out=mixT[:, 2:8, :], in_=TR2[:, 0:6, :], func=AF.Copy), writes=['mixT'], banks=[6])
            yield

        def out_proj(l, t):
            for hf in range(2):
                for k in range(8):
                    S.op('pe', lambda e, hf=hf, k=k: e.matmul(banks[2 + hf][:], lhsT=mixT[:, k, :], rhs=Wout[:, k, hf * 512:(hf + 1) * 512],
                                                             start=(k == 0), stop=(k == 7)),
                         reads=['mixT', 'Wout%d' % k], banks=[2 + hf], inc=(k == 7))
            yield
            X = xt(t)
            for hf in range(2):
                S.op('dve', lambda e, hf=hf: e.tensor_tensor(out=X[:, hf * 512:(hf + 1) * 512], in0=banks[2 + hf][:],
                                                            in1=X[:, hf * 512:(hf + 1) * 512], op=ALU.add),
                     reads=['x%d' % t], writes=['x%d' % t], banks=[2 + hf])
            yield

        def router(t, col):
            LG = TRF2[:, 0:NE]
            for k in range(8):
                S.op('pe', lambda e, k=k: e.matmul(LG, lhsT=hT[:, k, col * 128:(col + 1) * 128], rhs=routerw[:, k, :],
                                                  start=(k == 0), stop=(k == 7)),
                     reads=['hT', 'routerw'], banks=[6], inc=(k == 7))
            S.op('dve', lambda e: e.tensor_copy(out=lg[:], in_=LG), writes=['lg'], banks=[6])
            S.op('dve', lambda e: e.reduce_max(out=m1[:], in_=lg[:], axis=mybir.AxisListType.X), reads=['lg'], writes=['m1'])
            S.op('dve', lambda e: e.tensor_scalar(out=msk[:], in0=lg[:], scalar1=m1[:, 0:1], scalar2=-1e30, op0=ALU.is_equal, op1=ALU.mult),
                 reads=['lg', 'm1'], writes=['msk'])
            S.op('dve', lambda e: e.tensor_tensor(out=lg2[:], in0=lg[:], in1=msk[:], op=ALU.add), reads=['lg', 'msk'], writes=['lg2'])
            S.op('dve', lambda e: e.reduce_max(out=m2[:], in_=lg2[:], axis=mybir.AxisListType.X), reads=['lg2'], writes=['m2'])
            S.op('dve', lambda e: e.tensor_scalar(out=msk[:], in0=lg[:], scalar1=m2[:, 0:1], scalar2=None, op0=ALU.is_ge),
                 reads=['lg', 'm2'], writes=['msk'])
            S.op('dve', lambda e: e.tensor_scalar(out=nm1[:], in0=m1[:], scalar1=-1.0, scalar2=None, op0=ALU.mult),
                 reads=['m1'], writes=['nm1'])
            S.op('act', lambda e: e.activation(out=lg2[:], in_=lg[:], func=AF.Exp, bias=nm1[:, 0:1]), reads=['lg', 'nm1'], writes=['lg2'])
            S.op('act', lambda e: e.activation(out=rr[:], in_=m2[:], func=AF.Exp, bias=nm1[:, 0:1]), reads=['m2', 'nm1'], writes=['rr'])
            S.op('dve', lambda e: e.tensor_scalar(out=rr[:], in0=rr[:], scalar1=1.0, scalar2=None, op0=ALU.add), reads=['rr'], writes=['rr'])
            S.op('dve', lambda e: e.reciprocal(out=rr[:], in_=rr[:]), reads=['rr'], writes=['rr'])
            S.op('dve', lambda e: e.tensor_tensor(out=lg2[:], in0=lg2[:], in1=msk[:], op=ALU.mult), reads=['lg2', 'msk'], writes=['lg2'])
            S.op('dve', lambda e: e.tensor_scalar(out=comb[:, t, :], in0=lg2[:], scalar1=rr[:, 0:1], scalar2=None, op0=ALU.mult),
                 reads=['lg2', 'rr'], writes=['comb'])
            yield

        def state_outputs(l, t):
            if t == 17:
                fp32_T_out(lambda c: kf[:, c, :], lambda: k_p_d[l], (0, 128), 'kf', 0)
                S.dma('sp', lambda e: e.dma_start(out=v_p_d[l], in_=vf[:]), reads=['vf'], writes=[dout_res()], chan='o')
                fp32_T_out(lambda c: uextb[1][:, c, 16:144], lambda: pool_p_d[l], (0, 128), 'uext1', 1)
            if t == 18:
                fp32_T_out(lambda c: kf[:, c, :], lambda: k_s_new_d[l], (0, 128), 'kf', 0)
                S.dma('sp', lambda e: e.dma_start(out=v_s_new_d[l], in_=vf[:]), reads=['vf'], writes=[dout_res()], chan='o')
                for c in range(2):
                    S.op('pool', lambda e, c=c: e.tensor_copy(out=Wk[:, c, :].rearrange("p (b i) -> p b i", b=16), in_=us[:, c, :, 16:24]),
                         reads=['us'], writes=['Wk'])
                fp32_T_out(lambda c: Wk[:, c, :], lambda: pool_s_new_d[l], (0, 128), 'Wk', 1)

        def stage1(l, t):
            r = RSLOT(t)
            halo = t in (0, 1)
            kv_only = (l == 0 and t == 0) or (l == 1 and t == 1)
            want_state = t in (17, 18)
            yield from project(l, t, r, halo, want_state)
            if want_state:
                state_outputs(l, t)
                yield
            if not kv_only:
                yield from pool_mix(l, t)
            yield

        def stage2(l, t, col):
            r = RSLOT(t)
            kv_only = (l == 0 and t == 0) or (l == 1 and t == 1)
            if kv_only:
                return
            if 10 <= t <= 18:
                if t >= 11:
                    sample_k_transpose(l, t - 11)
                if t <= 17:
                    sample_kv_load(l, t - 10)
                yield
            if t == 18:
                yield from attention_sample(l)
            else:
                yield from attention_prompt(l, t, r)
            yield from attn_finish(l, t)
            if DEBUG and l == 0 and t == 18:
                S.dma('sp', lambda e: e.dma_start(out=dbg_attn, in_=mixtok[:]), reads=['mixtok'], writes=[dout_res()], chan='o')
                S.dma('sp', lambda e: e.dma_start(out=dbg_pool, in_=mixT[:, 0:2, :]), reads=['mixT'], writes=[dout_res()], chan='o')
            yield from pool_project(l, t)
            yield from out_proj(l, t)
            yield from norm_T(t, gffnT[:, l, :], hT[:, :, col * 128:(col + 1) * 128], 'hT', 1)
            if l == 1:
                yield from router(t, col)

        def run_interleaved(gens):
            active = list(gens)
            while active:
                for g_ in list(active):
                    try:
                        next(g_)
                    except StopIteration:
                        active.remove(g_)

        def ffn_load(wg_d, wu_d, wd_d, grp, slot):
            wg, wu, wd = slot_views(slot)
            ng = len(grp)
            f0 = grp[0] * 128
            sres = 'slot%d' % slot
            S.dma('pool', lambda e: e.dma_start(
                out=wg[:, :, 0:ng * 128], in_=wg_d.rearrange("(k p) n -> p k n", p=128)[:, :, f0:f0 + ng * 128]),
                writes=[sres], chan='w')
            S.dma('pool', lambda e: e.dma_start(
                out=wu[:, :, 0:ng * 128], in_=wu_d.rearrange("(k p) n -> p k n", p=128)[:, :, f0:f0 + ng * 128]),
                writes=[sres + 'u'], chan='w')
            S.dma('pool', lambda e: e.dma_start(
                out=wd[:, 0:ng, :], in_=wd_d[f0:f0 + ng * 128, :].rearrange("(g p) n -> p g n", p=128)),
                writes=[sres + 'd'], chan='w')

        def ffn_prefetch(l):
            S.op('pool', lambda e: e.memset(fence[:], 0.0), writes=['fence'] + ['Win%d' % k for k in range(8)])
            if l == 0:
                wg_d, wu_d, wd_d = fg_d[0], fu_d[0], fd_d[0]
            else:
                wg_d, wu_d, wd_d = mg_d[0, 0], mu_d[0, 0], md_d[0, 0]
            for gi_ in range(2):
                ffn_load(wg_d, wu_d, wd_d, list(range(gi_ * G, gi_ * G + G)), gi_)
            return 2

        def mixer_prefetch(l, last_slot):
            free = [s_ for s_ in range(NSLOT) if s_ != last_slot]
            names = []
            for s_ in free:
                names += ['slot%d' % s_, 'slot%du' % s_, 'slot%dd' % s_]
            S.op('pool', lambda e: e.memset(fence[:], 0.0), writes=['fence'] + names)
            skip = set()
            for k in range(8):
                if (k // 4) in free:
                    S.dma('pool', lambda e, k=k: e.dma_start(out=Win[:, k, :], in_=w_in_d[l, k * 128:(k + 1) * 128, :]),
                          writes=['Win%d' % k], chan='w')
                    skip.add(('in', k))
            for k in range(8):
                if k >= 6 or 2 in free:
                    S.dma('pool', lambda e, k=k: e.dma_start(out=Wout[:, k, :], in_=w_out_d[l, k * 128:(k + 1) * 128, :]),
                          writes=['Wout%d' % k], chan='w')
                    skip.add(('out', k))
            return skip

        def ffn_segment(l, tiles, prefetched=0):
            ne = 1 if l == 0 else NE
            ncol = len(tiles)
            blocks = [list(range(i, min(i + 3, ncol))) for i in range(0, ncol, 3)]
            groups = [list(range(i, min(i + G, NFF))) for i in range(0, NFF, G)]
            gi = 0
            gubuf = 0
            pending = [None]
            for ex in range(ne):
                if l == 0:
                    wg_d, wu_d, wd_d = fg_d[0], fu_d[0], fd_d[0]
                else:
                    wg_d, wu_d, wd_d = mg_d[0, ex], mu_d[0, ex], md_d[0, ex]
                for grp in groups:
                    slot = gi % NSLOT
                    wg, wu, wd = slot_views(slot)
                    ng = len(grp)
                    sres = 'slot%d' % slot
                    if gi >= prefetched:
                        ffn_load(wg_d, wu_d, wd_d, grp, slot)
                    gi += 1
                    def do_block(blk, wg=wg, wu=wu, wd=wd, ng=ng, sres=sres, ex=ex):
                        nonlocal gubuf
                        nt = len(blk)
                        c0 = blk[0] * 128
                        ntok = nt * 128
                        ab = (gubuf // 2) % 2
                        for fi in range(ng):
                            gb = gubuf % 2
                            gubuf += 1
                            for k in range(8):
                                S.op('pe', lambda e, fi=fi, k=k: e.matmul(banks[6][:, 0:ntok], lhsT=wg[:, k, fi * 128:(fi + 1) * 128],
                                                                          rhs=hT[:, k, c0:c0 + ntok], start=(k == 0), stop=(k == 7)),
                                     reads=['hT', sres], banks=[6], inc=(k == 7))
                            for k in range(8):
                                S.op('pe', lambda e, fi=fi, k=k: e.matmul(banks[7][:, 0:ntok], lhsT=wu[:, k, fi * 128:(fi + 1) * 128],
                                                                          rhs=hT[:, k, c0:c0 + ntok], start=(k == 0), stop=(k == 7)),
                                     reads=['hT', sres + 'u'], banks=[7], inc=(k == 7))
                            S.op('act', lambda e, gb=gb: e.activation(out=sg[gb][:, 0:ntok], in_=banks[6][:, 0:ntok], func=AF.Silu),
                                 writes=['sg%d' % gb], banks=[6])
                            S.op('dve', lambda e, gb=gb, fi=fi, ab=ab: e.tensor_tensor(out=aT[ab][:, fi, 0:ntok], in0=banks[7][:, 0:ntok],
                                                                                      in1=sg[gb][:, 0:ntok], op=ALU.mult),
                                 reads=['sg%d' % gb], writes=['aT%d_%d' % (ab, fi)], banks=[7])
                        def down():
                            for j in range(nt):
                                for hf in range(2):
                                    for fi in range(ng):
                                        S.op('pe', lambda e, j=j, hf=hf, fi=fi, ab=ab: e.matmul(
                                            banks[2 * j + hf][:], lhsT=aT[ab][:, fi, j * 128:(j + 1) * 128], rhs=wd[:, fi, hf * 512:(hf + 1) * 512],
                                            start=(fi == 0), stop=(fi == ng - 1)),
                                            reads=['aT%d_%d' % (ab, fi), sres + 'd'], banks=[2 * j + hf], inc=(fi == ng - 1))
                                t = tiles[blk[j]]
                                X = xt(t)
                                for hf in range(2):
                                    if l == 0:
                                        S.op('dve', lambda e, j=j, hf=hf, X=X: e.tensor_tensor(
                                            out=X[:, hf * 512:(hf + 1) * 512], in0=banks[2 * j + hf][:], in1=X[:, hf * 512:(hf + 1) * 512], op=ALU.add),
                                            reads=['x%d' % t], writes=['x%d' % t], banks=[2 * j + hf])
                                    else:
                                        S.op('dve', lambda e, j=j, hf=hf, X=X, t=t, ex=ex: e.scalar_tensor_tensor(
                                            out=X[:, hf * 512:(hf + 1) * 512], in0=banks[2 * j + hf][:], scalar=comb[:, t, ex:ex + 1],
                                            in1=X[:, hf * 512:(hf + 1) * 512], op0=ALU.mult, op1=ALU.add),
                                            reads=['x%d' % t, 'comb'], writes=['x%d' % t], banks=[2 * j + hf])
                        if pending[0] is not None:
                            pending[0]()
                        pending[0] = down
                    for blk in blocks:
                        do_block(blk)
            if pending[0] is not None:
                pending[0]()
                pending[0] = None

        def y_out(ts):
            for t in ts:
                S.dma('sp', lambda e, t=t: e.dma_start(out=y_d[(t - 2) * 128:(t - 1) * 128, :], in_=xt(t)), reads=['x%d' % t],
                      writes=[dout_res()], chan='o')

        S.barrier()
        for t in range(1, NTILE):
            S.dma('sp', lambda e, t=t: e.dma_start(out=xt(t), in_=xin[t * 128:(t + 1) * 128, :]), writes=['x%d' % t], chan='x')
        S.dma('pool', lambda e: e.dma_start(out=routerw[:], in_=router_d[0].rearrange("(k p) n -> p k n", p=128)),
              writes=['routerw'], chan='w')

        def old_state_copies():
            for l in range(2):
                S.dma('sp', lambda e, l=l: e.dma_start(out=k_s_old_d[l], in_=sk_d[l, :, 8:128, :]), writes=[dout_res()], chan='o')
                S.dma('sp', lambda e, l=l: e.dma_start(out=v_s_old_d[l], in_=sv_d[l, :, 8:128, :]), writes=[dout_res()], chan='o')
                S.dma('sp', lambda e, l=l: e.dma_start(out=pool_s_old_d[l], in_=sp_d[l].rearrange("(b j) d -> b j d", j=15)[:, 8:15, :]),
                      writes=[dout_res()], chan='o')
        nxt_skip = set()
        for l in range(2):
            for si, seg in enumerate(SEGS):
                tiles = [t for t in seg if not (l == 1 and t == 0)]
                if not (l == 0 and si == 0):
                    load_mixer_weights(l, nxt_skip)
                if 18 in tiles:
                    for hb in range(2):
                        S.dma('sp', lambda e, hb=hb, l=l: e.dma_start(out=sptok[:, hb, :], in_=sp_d[l, hb * 120:(hb + 1) * 120, :]),
                              writes=['sptok'], chan='x')
                    for hb in range(2):
                        for c in range(2):
                            S.op('pe', lambda e, hb=hb, c=c: e.transpose(out=TRF[:, 0:120], in_=sptok[:, hb, c * 128:(c + 1) * 128],
                                                                        identity=identf[0:120, 0:120]),
                                 reads=['sptok', 'identf'], banks=[5])
                            S.op('dve', lambda e, hb=hb, c=c: e.tensor_copy(
                                out=us[:, c, hb * 8:(hb + 1) * 8, 1:16], in_=TRF[:, 0:120].rearrange("p (b j) -> p b j", j=15)),
                                writes=['us'], banks=[5])
                ffn_tiles = []
                cols = {}
                for t in tiles:
                    kv_only = (l == 0 and t == 0) or (l == 1 and t == 1)
                    cols[t] = len(ffn_tiles)
                    if not kv_only:
                        ffn_tiles.append(t)
                run_interleaved([stage1(l, tiles[0])])
                for i in range(1, len(tiles)):
                    run_interleaved([stage2(l, tiles[i - 1], cols[tiles[i - 1]]), stage1(l, tiles[i])])
                npre = ffn_prefetch(l)
                run_interleaved([stage2(l, tiles[-1], cols[tiles[-1]])])
                S.barrier()
                if DEBUG:
                    for t in [tt for tt in ffn_tiles if tt in (17, 18)]:
                        S.dma('sp', lambda e, t=t, l=l: e.dma_start(out=dbg_d[2 * l, (t - 17) * 128:(t - 16) * 128, :], in_=xt(t)),
                              reads=['x%d' % t], writes=[dout_res()], chan='o')
                if l == 0 and si == 0:
                    old_state_copies()
                ffn_segment(l, ffn_tiles, npre)
                nl, nsi = (l, si + 1) if si + 1 < len(SEGS) else (l + 1, 0)
                nxt_skip = set()
                if nl < 2:
                    nxt_skip = mixer_prefetch(nl, ((1 if l == 0 else NE) * ((NFF + G - 1) // G) - 1) % NSLOT)
                if l == 1 and si == len(SEGS) - 1:
                    y_out(ffn_tiles)
                S.barrier()
                if l == 1 and si < len(SEGS) - 1:
                    y_out(ffn_tiles)
                if DEBUG:
                    for t in [tt for tt in ffn_tiles if tt in (17, 18)]:
                        S.dma('sp', lambda e, t=t, l=l: e.dma_start(out=dbg_d[2 * l + 1, (t - 17) * 128:(t - 16) * 128, :], in_=xt(t)),
                              reads=['x%d' % t], writes=[dout_res()], chan='o')
        S.barrier()
        S.emit()
        S.close()
    return nc


_NC_CACHE = {}
_DBG = {}


def _consts():
    b = np.arange(128)[:, None].astype(np.float64)
    a = np.arange(128)[None, :].astype(np.float64)
    cd = np.zeros((128, 392), np.float32)
    cv = np.zeros((128, 392), np.float32)
    cd[:, 0:128] = a - b + 128
    cv[:, 0:128] = (a < b)
    cd[:, 128:256] = np.maximum(a - b, 0)
    cv[:, 128:256] = (a >= b)
    i8 = np.arange(8)[None, :].astype(np.float64)
    cd[:, 256:264] = 128 + i8 - b
    cv[:, 256:264] = (b > i8)
    kb, kj = np.arange(128)[:, None] // 8, np.arange(128)[:, None] % 8
    qb, qi = np.arange(128)[None, :] // 8, np.arange(128)[None, :] % 8
    cd[:, 264:392] = np.maximum(qi - kj, 0)
    cv[:, 264:392] = (kb == qb) & (kj <= qi)
    return cd, cv


def _tab(first_half):
    wins = [2, 4, 8, 16]
    tab = np.zeros((128, 2, 2, 128), np.float32)
    for c in range(2):
        for hh in range(2):
            w = wins[c * 2 + hh]
            tab[hh * 64:(hh + 1) * 64, c, 1, :] = 1.0 / w
            if first_half:
                cnt = np.minimum(w, np.arange(128) + 1).astype(np.float32)
                tab[hh * 64:(hh + 1) * 64, c, 0, :] = 1.0 / cnt
            else:
                tab[hh * 64:(hh + 1) * 64, c, 0, :] = 1.0 / w
    return tab


def kernel(x_prompt, x_sample, state_pool, state_win_k, state_win_v,
           norm_mix, w_in, q_norm, k_norm, attn_sinks, pool_w, pool_scale, w_out, norm_ffn,
           ffn_w_gate, ffn_w_up, ffn_w_down, moe_router, moe_w_gate, moe_w_up, moe_w_down):
    f = lambda a: np.ascontiguousarray(np.asarray(a, dtype=np.float32))
    x_prompt, x_sample = f(x_prompt), f(x_sample)
    state_pool, state_win_k, state_win_v = f(state_pool), f(state_win_k), f(state_win_v)
    w_in = f(w_in)
    qcols = []
    for (ha, hb) in QPAIR:
        qcols += list(range(256 + ha * 64, 256 + ha * 64 + 64)) + list(range(256 + hb * 64, 256 + hb * 64 + 64))
    cols = list(range(256)) + qcols + list(range(1024, 1536))
    w_in_p = np.ascontiguousarray(w_in[:, :, cols])
    cd, cv = _consts()
    shared = {
        "cdist": cd, "cvalid": cv,
        "norm_mix": f(norm_mix), "norm_ffn": f(norm_ffn), "w_in": w_in_p, "q_norm": f(q_norm), "k_norm": f(k_norm),
        "attn_sinks": f(attn_sinks), "pool_w": f(pool_w), "pool_scale": f(pool_scale), "w_out": f(w_out),
        "ffn_w_gate": f(ffn_w_gate), "ffn_w_up": f(ffn_w_up), "ffn_w_down": f(ffn_w_down),
        "moe_router": f(moe_router), "moe_w_gate": f(moe_w_gate), "moe_w_up": f(moe_w_up), "moe_w_down": f(moe_w_down),
    }
    in_maps = []
    for c in range(NCORE):
        seq, half = c // 2, c % 2
        main = x_prompt[seq, half * 2048:(half + 1) * 2048]
        halo = x_prompt[seq, 2048 - 256:2048] if half == 1 else np.zeros((256, D), np.float32)
        xs_ = x_sample[c * 16:(c + 1) * 16].reshape(128, D)
        m = dict(shared)
        m["xin"] = np.ascontiguousarray(np.concatenate([halo, main, xs_], 0))
        m["flag"] = np.full((128, 1), float(half), np.float32)
        m["tab"] = _tab(half == 0)
        m["st_pool"] = np.ascontiguousarray(state_pool[:, c * 16:(c + 1) * 16].reshape(2, 240, 256))
        m["st_k"] = np.ascontiguousarray(state_win_k[:, c * 16:(c + 1) * 16].reshape(2, 16, 128, 256))
        m["st_v"] = np.ascontiguousarray(state_win_v[:, c * 16:(c + 1) * 16].reshape(2, 16, 128, 256))
        in_maps.append(m)
    if "nc" not in _NC_CACHE:
        _NC_CACHE["nc"] = build()
    res = run_bass_kernel_spmd(_NC_CACHE["nc"], in_maps, core_ids=list(range(NCORE)))
    R = res.results
    if DEBUG:
        _DBG["dbg"] = [np.asarray(r["dbg"]) for r in R]
        _DBG["attn"] = [np.asarray(r["dbg_attn"]).astype(np.float32) for r in R]
        _DBG["pool"] = [np.asarray(r["dbg_pool"]).astype(np.float32) for r in R]
    y_prompt = np.zeros((4, 4096, D), np.float32)
    y_sample = np.zeros((128, 8, D), np.float32)
    pool_p = np.zeros((2, 4, 15, 256), np.float32)
    k_p = np.zeros((2, 4, 128, 4, 64), np.float32)
    v_p = np.zeros((2, 4, 128, 4, 64), np.float32)
    pool_s = np.zeros((2, 128, 15, 256), np.float32)
    k_s = np.zeros((2, 128, 128, 4, 64), np.float32)
    v_s = np.zeros((2, 128, 128, 4, 64), np.float32)
    for c in range(NCORE):
        seq, half = c // 2, c % 2
        r = R[c]
        y = np.asarray(r["y"])
        y_prompt[seq, half * 2048:(half + 1) * 2048] = y[0:2048]
        y_sample[c * 16:(c + 1) * 16] = y[2048:2176].reshape(16, 8, D)
        if half == 1:
            pool_p[:, seq] = np.asarray(r["pool_p"])[:, 113:128, :]
            k_p[:, seq] = np.asarray(r["k_p"]).reshape(2, 128, 4, 64)
            v_p[:, seq] = np.asarray(r["v_p"]).reshape(2, 128, 4, 64)
        sl = slice(c * 16, (c + 1) * 16)
        pool_s[:, sl, 0:7] = np.asarray(r["pool_s_old"])
        pool_s[:, sl, 7:15] = np.asarray(r["pool_s_new"]).reshape(2, 16, 8, 256)
        k_s[:, sl, 0:120] = np.asarray(r["k_s_old"]).reshape(2, 16, 120, 4, 64)
        k_s[:, sl, 120:128] = np.asarray(r["k_s_new"]).reshape(2, 16, 8, 4, 64)
        v_s[:, sl, 0:120] = np.asarray(r["v_s_old"]).reshape(2, 16, 120, 4, 64)
        v_s[:, sl, 120:128] = np.asarray(r["v_s_new"]).reshape(2, 16, 8, 4, 64)
    return (y_prompt, y_sample, pool_p, k_p, v_p, pool_s, k_s, v_s)
```
